# Optimizing a Trainium2 kernel written in Bass

```python
import math
import jax, jax.numpy as jnp
from jax import lax
import numpy as np


D_MODEL = 1024
BATCH = 2
SEQ = 8192
DEPTH = 1

MEM_LEN = 256
CONV_CH = 512
CONV_WIDTH = 31
N_HEADS = 8
N_KV_HEADS = 2
HEAD_DIM = 64
GQ = N_HEADS // N_KV_HEADS
ATTN_W = N_HEADS * HEAD_DIM
KV_W = N_KV_HEADS * HEAD_DIM
WINDOW = 128
BLOCK = 128
D_MIX = CONV_CH + ATTN_W
D_IN = 2 * CONV_CH + ATTN_W + 2 * KV_W
MEM_HEADS = 4
MEM_HEAD_DIM = D_MODEL // MEM_HEADS
N_GROUPS = 4
EXPERTS_PER_GROUP = 4
N_EXPERTS = N_GROUPS * EXPERTS_PER_GROUP
TOP_K = 2
D_EXPERT = D_MODEL // 2
ALPHA = (2.0 * DEPTH) ** 0.25
BETA = (8.0 * DEPTH) ** -0.25
LN_EPS = 1e-5

kernel_name = 'hybrid_conformer_swa_sink_memory_hmoe'


def layer_norm(x, g, b):
    xf = x.astype(jnp.float32)
    mu = jnp.mean(xf, axis=-1, keepdims=True)
    var = jnp.mean(jnp.square(xf - mu), axis=-1, keepdims=True)
    return ((xf - mu) * lax.rsqrt(var + LN_EPS) * g + b).astype(x.dtype)


def conformer_conv(u, w_dw, b_dw, g_cn, b_cn):
    a, gate = jnp.split(u, 2, axis=-1)
    h = a * jax.nn.sigmoid(gate)
    h = lax.conv_general_dilated(
        h, w_dw[:, None, :].astype(h.dtype), window_strides=(1,),
        padding=[(CONV_WIDTH - 1, 0)],
        dimension_numbers=('NWC', 'WIO', 'NWC'),
        feature_group_count=h.shape[-1]) + b_dw
    h = layer_norm(h, g_cn, b_cn)
    return jax.nn.silu(h)


def sliding_window_sink_attention(q, k, v, sinks):
    B, S, _ = q.shape
    nb = S // BLOCK
    qb = q.reshape(B, nb, BLOCK, N_KV_HEADS, GQ, HEAD_DIM)

    def band(t):
        t = t.reshape(B, S, N_KV_HEADS, HEAD_DIM)
        t = jnp.pad(t, ((0, 0), (BLOCK, 0), (0, 0), (0, 0)))
        t = t.reshape(B, nb + 1, BLOCK, N_KV_HEADS, HEAD_DIM)
        return jnp.concatenate([t[:, :-1], t[:, 1:]], axis=2)

    kb, vb = band(k), band(v)
    s = jnp.einsum('bnqhgd,bnkhd->bnhgqk', qb, kb).astype(jnp.float32) * (HEAD_DIM ** -0.5)
    qi = jnp.arange(BLOCK)[:, None]
    kj = jnp.arange(2 * BLOCK)[None, :]
    dist = qi + BLOCK - kj
    blk = jnp.arange(nb)[:, None, None]
    valid = (dist >= 0) & (dist < WINDOW) & (blk * BLOCK - BLOCK + kj >= 0)
    s = jnp.where(valid[None, :, None, None], s, -jnp.inf)
    sink = jnp.broadcast_to(sinks.astype(jnp.float32).reshape(1, 1, N_KV_HEADS, GQ, 1, 1),
                            s.shape[:-1] + (1,))
    p = jax.nn.softmax(jnp.concatenate([s, sink], axis=-1), axis=-1)[..., :-1]
    o = jnp.einsum('bnhgqk,bnkhd->bnqhgd', p.astype(vb.dtype), vb)
    return o.reshape(B, S, ATTN_W)


def memory_cross_attention(x, mem, w_q, w_kv, w_o):
    B, S, _ = x.shape
    q = (x @ w_q).reshape(B, S, MEM_HEADS, MEM_HEAD_DIM)
    k, v = jnp.split(mem @ w_kv, 2, axis=-1)
    k = k.reshape(B, -1, MEM_HEADS, MEM_HEAD_DIM)
    v = v.reshape(B, -1, MEM_HEADS, MEM_HEAD_DIM)
    s = jnp.einsum('bshd,bmhd->bhsm', q, k).astype(jnp.float32) * (MEM_HEAD_DIM ** -0.5)
    p = jax.nn.softmax(s, axis=-1).astype(v.dtype)
    o = jnp.einsum('bhsm,bmhd->bshd', p, v).reshape(B, S, D_MODEL)
    return o @ w_o


def hierarchical_moe(x, w_group, b_group, w_router, b_router, w_gate, w_up, w_down):
    B, S, D = x.shape
    t = x.reshape(B * S, D)
    g_prob = jax.nn.softmax((t @ w_group).astype(jnp.float32) + b_group, axis=-1)
    g_p, g_idx = lax.top_k(g_prob, 1)
    e_logits = jnp.einsum('td,gde->tge', t, w_router).astype(jnp.float32) + b_router
    e_logits = jnp.take_along_axis(e_logits, g_idx[:, :, None], axis=1)[:, 0]
    e_top, e_idx = lax.top_k(e_logits, TOP_K)
    gate = g_p * jax.nn.softmax(e_top, axis=-1)
    expert_id = g_idx * EXPERTS_PER_GROUP + e_idx
    combine = jnp.sum(jax.nn.one_hot(expert_id, N_EXPERTS, dtype=jnp.float32) * gate[..., None], axis=1)
    h = jax.nn.silu(jnp.einsum('td,edf->tef', t, w_gate)) * jnp.einsum('td,edf->tef', t, w_up)
    h = h * combine[..., None].astype(h.dtype)
    y = jnp.einsum('tef,efd->td', h, w_down)
    return y.reshape(B, S, D)


def setup_inputs(seed: int = 0) -> dict:
    key = jax.random.key(seed)
    ks = jax.random.split(key, 26)
    L = DEPTH

    def nrm(k, shape, scale):
        return jax.random.normal(k, shape, jnp.float32) * scale

    return {
        'x': nrm(ks[0], (BATCH, SEQ, D_MODEL), 1.0),
        'mem': nrm(ks[1], (BATCH, MEM_LEN, D_MODEL), 1.0),
        'w_in': nrm(ks[2], (L, D_MODEL, D_IN), D_MODEL ** -0.5),
        'b_in': nrm(ks[3], (L, D_IN), 0.01),
        'w_dw': nrm(ks[4], (L, CONV_WIDTH, CONV_CH), CONV_WIDTH ** -0.5),
        'b_dw': nrm(ks[5], (L, CONV_CH), 0.01),
        'g_conv_norm': 1.0 + nrm(ks[6], (L, CONV_CH), 0.02),
        'b_conv_norm': nrm(ks[7], (L, CONV_CH), 0.01),
        'attn_sinks': nrm(ks[8], (L, N_HEADS), 0.5),
        'w_out': nrm(ks[9], (L, D_MIX, D_MODEL), BETA * D_MIX ** -0.5),
        'g_ln1': 1.0 + nrm(ks[10], (L, D_MODEL), 0.02),
        'b_ln1': nrm(ks[11], (L, D_MODEL), 0.01),
        'w_mq': nrm(ks[12], (L, D_MODEL, D_MODEL), D_MODEL ** -0.5),
        'w_mkv': nrm(ks[13], (L, D_MODEL, 2 * D_MODEL), D_MODEL ** -0.5),
        'w_mo': nrm(ks[14], (L, D_MODEL, D_MODEL), BETA * D_MODEL ** -0.5),
        'g_ln2': 1.0 + nrm(ks[15], (L, D_MODEL), 0.02),
        'b_ln2': nrm(ks[16], (L, D_MODEL), 0.01),
        'w_group': nrm(ks[17], (L, D_MODEL, N_GROUPS), D_MODEL ** -0.5),
        'b_group': nrm(ks[18], (L, N_GROUPS), 0.01),
        'w_router': nrm(ks[19], (L, N_GROUPS, D_MODEL, EXPERTS_PER_GROUP), D_MODEL ** -0.5),
        'b_router': nrm(ks[20], (L, N_GROUPS, EXPERTS_PER_GROUP), 0.01),
        'w_gate': nrm(ks[21], (L, N_EXPERTS, D_MODEL, D_EXPERT), D_MODEL ** -0.5),
        'w_up': nrm(ks[22], (L, N_EXPERTS, D_MODEL, D_EXPERT), D_MODEL ** -0.5),
        'w_down': nrm(ks[23], (L, N_EXPERTS, D_EXPERT, D_MODEL), BETA * D_EXPERT ** -0.5),
        'g_ln3': 1.0 + nrm(ks[24], (L, D_MODEL), 0.02),
        'b_ln3': nrm(ks[25], (L, D_MODEL), 0.01),
    }


def reference(x, mem, w_in, b_in, w_dw, b_dw, g_conv_norm, b_conv_norm, attn_sinks, w_out,
              g_ln1, b_ln1, w_mq, w_mkv, w_mo, g_ln2, b_ln2, w_group, b_group, w_router,
              b_router, w_gate, w_up, w_down, g_ln3, b_ln3):
    splits = [2 * CONV_CH, 2 * CONV_CH + ATTN_W, 2 * CONV_CH + ATTN_W + KV_W]
    for l in range(DEPTH):
        u = x @ w_in[l] + b_in[l]
        u_conv, q, k, v = jnp.split(u, splits, axis=-1)
        y_conv = conformer_conv(u_conv, w_dw[l], b_dw[l], g_conv_norm[l], b_conv_norm[l])
        y_attn = sliding_window_sink_attention(q, k, v, attn_sinks[l])
        mix = jnp.concatenate([y_conv, y_attn], axis=-1) @ w_out[l]
        x = layer_norm(ALPHA * x + mix, g_ln1[l], b_ln1[l])
        x = layer_norm(ALPHA * x + memory_cross_attention(x, mem, w_mq[l], w_mkv[l], w_mo[l]),
                       g_ln2[l], b_ln2[l])
        x = layer_norm(ALPHA * x + hierarchical_moe(x, w_group[l], b_group[l], w_router[l], b_router[l],
                                                    w_gate[l], w_up[l], w_down[l]),
                       g_ln3[l], b_ln3[l])
    return x
```

```python
import numpy as np
from contextlib import ExitStack
import concourse.bass as bass
import concourse.mybir as mybir
from concourse.bass_utils import run_bass_kernel_spmd

F32, BF16 = mybir.dt.float32, mybir.dt.bfloat16
AF = mybir.ActivationFunctionType
ALU = mybir.AluOpType
AX = mybir.AxisListType

NCORES = 8
D = 1024
SEQ = 8192
TOK = 2048
NG = 4
GT = 512
HALO = 128
XW = HALO + TOK
ALPHA = 2.0 ** 0.25
EPS = 1e-5
NEG = -30000.0
NSEM = 44
DEBUG = False


class T:
    __slots__ = ("ap", "w", "r", "dsem", "dcnt", "name", "wsame")

    def __init__(self, ap=None, name=""):
        self.ap = ap
        self.w = None
        self.r = {}
        self.dsem = None
        self.dcnt = 0
        self.name = name


class Eng:
    def __init__(self, name, sem):
        self.name = name
        self.sem = sem
        self.count = 0
        self.observed = {}
        self.ops = []


class Sched:
    def __init__(self, nc, stack):
        self.nc = nc
        self.stack = stack
        self.semn = 0
        self.sem_pool = [self.stack.enter_context(self.nc.semaphore(f"s{i}")) for i in range(NSEM)]
        self.engs = {}
        for n in ("pe", "act", "dve", "pool", "sp"):
            self.engs[n] = Eng(n, self.newsem("e_" + n))
        self.pending_dma = {}
        pool = list(self.sem_pool)
        with self.nc.Block() as block:
            @block.sync
            def _(h):
                for sm in pool:
                    h.sem_clear(sm)

    def newsem(self, name):
        self.semn += 1
        return self.sem_pool[self.semn - 1]

    def _collect(self, e, reads, writes):
        deps = {}

        def add(tok, raw):
            if tok is None:
                return
            sem, val = tok
            if sem is e.sem:
                if e.name == "pe":
                    return
            k = sem.name if hasattr(sem, "name") else id(sem)
            k = id(sem)
            if e.observed.get(k, 0) >= val:
                return
            if k not in deps or deps[k][1] < val:
                deps[k] = (sem, val)

        for t in reads:
            add(t.w, True)
        for t in writes:
            add(t.w, False)
            for tok in t.r.values():
                add(tok, False)
        waits = list(deps.values())
        for sem, val in waits:
            e.observed[id(sem)] = val
        return waits

    def op(self, eng, fn, reads=(), writes=(), inc=True, attach=True):
        e = self.engs[eng]
        waits = self._collect(e, reads, writes)
        if inc:
            e.count += 1
            tok = (e.sem, e.count)
        else:
            tok = (e.sem, e.count + 1)
        for t in reads:
            t.r[id(e.sem)] = tok
        for t in writes:
            t.w = tok
            t.r = {}
        esem = e.sem
        can_attach = attach and eng in ("act", "dve", "pool")

        def emit(h):
            ws = list(waits)
            last = ws.pop() if (ws and can_attach) else None
            for sem, val in ws:
                h.wait_ge(sem, val)
            ins = fn(h)
            if last is not None:
                ins._wait_ge(last[0], last[1])
            if inc:
                ins.then_inc(esem, 1)

        e.ops.append(emit)

    def dma(self, queue, out_ap, in_ap, reads=(), writes=(), sem_tile=None, **kw):
        e = self.engs[queue]
        waits = self._collect(e, reads, writes)
        st = sem_tile
        if st.dsem is None:
            st.dsem = self.newsem("d")
        st.dcnt += 1
        tok = (st.dsem, 16 * st.dcnt)
        for t in reads:
            t.r[id(st.dsem)] = tok
        for t in writes:
            t.w = tok
            t.r = {}
        self.pending_dma[id(st.dsem)] = tok
        dsem = st.dsem

        def emit(h):
            for sem, val in waits:
                h.wait_ge(sem, val)
            h.dma_start(out=out_ap, in_=in_ap, **kw).then_inc(dsem, 16)

        e.ops.append(emit)

    def wait_all_dma(self, eng="sp"):
        e = self.engs[eng]
        toks = list(self.pending_dma.values())
        self.pending_dma = {}

        def emit(h):
            for sem, val in toks:
                h.wait_ge(sem, val)

        e.ops.append(emit)

    def flush(self):
        self.wait_all_dma("sp")
        with self.nc.Block() as block:
            for name, deco in (("pool", block.gpsimd), ("pe", block.tensor), ("act", block.scalar),
                               ("dve", block.vector), ("sp", block.sync)):
                ops = self.engs[name].ops

                def body(h, ops=ops):
                    for f in ops:
                        f(h)

                deco(body)
                self.engs[name].ops = []


def build_program(stop_after=None):
    nc = bass.Bass("TRN2", target_bir_lowering=False)
    dt = nc.dram_tensor

    def din(name, shape, dtype=F32):
        return dt(name, list(shape), dtype, kind="ExternalInput").ap()

    xT = din("xT", [D, XW])
    flag = din("flag", [128, 1])
    memT = din("memT", [D, 256])
    w_in = din("w_in", [D, 1792])
    b_in = din("b_in", [128, 14])
    bv_rep = din("bv_rep", [128, 128])
    wdw = din("wdw", [128, 4 * 31])
    cpar = din("cpar", [128, 12])
    sinks = din("sinks", [128, 4])
    w_out = din("w_out", [D, D])
    lnp = din("lnp", [128, 48])
    w_mq = din("w_mq", [D, D])
    w_mkv = din("w_mkv", [D, 2 * D])
    w_mo = din("w_mo", [D, D])
    w_rt = din("w_rt", [128, 8 * 20])
    b_rt = din("b_rt", [128, 20])
    b_rt_col = din("b_rt_col", [20, 1])
    w_gate = din("w_gate", [16, D, 512])
    w_up = din("w_up", [16, D, 512])
    w_down = din("w_down", [16, 512, D])
    masks = din("masks", [128, 3 * 512])
    consts = din("consts", [128, 6 * 128])
    identf = din("identf", [128, 128])
    sel = din("sel", [16, 16 * 128])
    outT = dt("outT", [D, TOK], F32, kind="ExternalOutput").ap()
    x1T = dt("x1T", [D, TOK], F32, kind="ExternalOutput" if DEBUG else "Internal").ap()

    with ExitStack() as stack:
        S = Sched(nc, stack)
        ec = stack.enter_context

        def sb(name, shape, dtype, st=None):
            return (st or stack).enter_context(nc.sbuf_tensor(name, list(shape), dtype))

        banks = []
        for i in range(8):
            p = ec(nc.psum_tensor(f"ps{i}", [128, 512], F32))
            banks.append((p, T(name=f"ps{i}")))
        bstate = {"i": 0}

        def bank():
            b = banks[bstate["i"] % 8]
            bstate["i"] += 1
            return b

        def mm(out_ap, outT_, lhsT, rhs, reads, start, stop):
            S.op("pe", lambda h: h.matmul(out_ap, lhsT=lhsT, rhs=rhs, start=start, stop=stop),
                 reads=reads, writes=[outT_], inc=stop)

        ymix = sb("ymix", [128, 8, TOK], BF16)
        ymT = [[T(name=f"ym{c}_{g}") for g in range(NG)] for c in range(8)]
        par_b_in = sb("p_b_in", [128, 14], F32)
        par_cpar = sb("p_cpar", [128, 12], F32)
        par_sinks = sb("p_sinks", [128, 4], F32)
        par_lnp = sb("p_lnp", [128, 48], F32)
        par_lnpa = sb("p_lnpa", [128, 16], F32)
        par_flag = sb("p_flag", [128, 1], F32)
        par_bv = sb("p_bv", [128, 128], F32)
        par_wdw = sb("p_wdw", [128, 4 * 31], F32)
        par_brt = sb("p_brt", [128, 20], F32)
        par_brc = sb("p_brc", [20, 1], F32)
        cst = sb("cst", [128, 6 * 128], BF16)
        idf = sb("idf", [128, 128], F32)
        par_t = T(name="params")
        ident_b = cst[:, 0:128]
        ones0 = cst[:, 128:256]
        ones1 = cst[:, 256:384]
        ones512 = cst[:, 384:512]
        ones1024 = cst[:, 512:640]
        onesP = cst[:, 640:768]

        plist = [(par_b_in, b_in, "sp"), (par_cpar, cpar, "sp"), (par_sinks, sinks, "sp"), (par_lnp, lnp, "sp"),
                 (par_flag, flag, "sp"), (par_bv, bv_rep, "sp"), (par_wdw, wdw, "sp"), (par_brt, b_rt, "sp"), (par_brc, b_rt_col, "sp"),
                 (idf, identf, "sp"), (cst, consts, "pool")]
        cst_t = T(name="consts")
        for dst, src, q in plist:
            S.dma(q, dst[:], src, writes=[], sem_tile=(par_t if q == "sp" else cst_t))
        par_t.w = (par_t.dsem, 16 * par_t.dcnt)
        cst_t.w = (cst_t.dsem, 16 * cst_t.dcnt)
        sinkexp = sb("sinkexp", [128, 4], F32)
        sinkexp_t = T(name="sinkexp")
        S.op("act", lambda h: h.activation(out=sinkexp[:], in_=par_sinks[:], func=AF.Exp),
             reads=[par_t], writes=[sinkexp_t])
        lnpa_t = T(name="lnpa")
        S.op("dve", lambda h: h.tensor_scalar(par_lnpa[:], par_lnp[:, 16:32], ALPHA, None, op0=ALU.mult),
             reads=[par_t], writes=[lnpa_t])

        def colsl(c):
            return slice(c * 128, (c + 1) * 128)

        def layernorm(z_ap, z_t, nch, ones_ap, outs, tmp, ntok=GT, post=None):
            for c in range(nch):
                ln_pre(z_ap, z_t, c, tmp, ntok)
            ln_post(z_ap, z_t, nch, ones_ap, outs, tmp, ntok, post)

        def ln_pre(z_ap, z_t, c, tmp, ntok=GT, cast_eng="dve"):
            zb, zb_t, z2b, z2b_t = tmp[0:4]
            if cast_eng == "act":
                S.op("act", lambda h, c=c: h.activation(out=zb[:, c, :ntok], in_=z_ap(c), func=AF.Copy),
                     reads=[z_t(c)], writes=[zb_t[c]])
            else:
                S.op("dve", lambda h, c=c: h.tensor_copy(out=zb[:, c, :ntok], in_=z_ap(c)),
                     reads=[z_t(c)], writes=[zb_t[c]])
            S.op("act", lambda h, c=c: h.activation(out=z2b[:, c, :ntok], in_=z_ap(c), func=AF.Square),
                 reads=[z_t(c)], writes=[z2b_t[c]])

        def ln_post(z_ap, z_t, nch, ones_ap, outs, tmp, ntok=GT, post=None):
            zb, zb_t, z2b, z2b_t, rstd, rstd_t, nmr, nmr_t, msq, msq_t, tt_, tt_t = tmp
            pm, pm_t = bank()
            for c in range(nch):
                mm(pm[:, :ntok], pm_t, ones_ap, zb[:, c, :ntok], [zb_t[c], cst_t], c == 0, c == nch - 1)
            pe2, pe2_t = bank()
            for c in range(nch):
                mm(pe2[:, :ntok], pe2_t, ones_ap, z2b[:, c, :ntok], [z2b_t[c], cst_t], c == 0, c == nch - 1)
            S.op("act", lambda h: h.activation(out=msq[:, :ntok], in_=pm[:, :ntok], func=AF.Square),
                 reads=[pm_t], writes=[msq_t])
            S.op("dve", lambda h: h.scalar_tensor_tensor(out=rstd[:, :ntok], in0=pe2[:, :ntok], scalar=EPS,
                                                         in1=msq[:, :ntok], op0=ALU.add, op1=ALU.subtract),
                 reads=[pe2_t, msq_t], writes=[rstd_t])
            S.op("act", lambda h: h.activation(out=rstd[:, :ntok], in_=rstd[:, :ntok], func=AF.Ln),
                 reads=[rstd_t], writes=[rstd_t])
            S.op("act", lambda h: h.activation(out=rstd[:, :ntok], in_=rstd[:, :ntok], func=AF.Exp, scale=-0.5),
                 reads=[rstd_t], writes=[rstd_t])
            S.op("dve", lambda h: h.scalar_tensor_tensor(out=nmr[:, :ntok], in0=pm[:, :ntok], scalar=-1.0,
                                                         in1=rstd[:, :ntok], op0=ALU.mult, op1=ALU.mult),
                 reads=[pm_t, rstd_t], writes=[nmr_t])
            for c in range(nch):
                k = c % len(tt_t)
                S.op("dve", lambda h, c=c, k=k: h.tensor_tensor(out=tt_[:, k, :ntok], in0=z_ap(c),
                                                                in1=rstd[:, :ntok], op=ALU.mult),
                     reads=[z_t(c), rstd_t], writes=[tt_t[k]])
                S.op("pool", lambda h, k=k: h.tensor_tensor(out=tt_[:, k, :ntok], in0=tt_[:, k, :ntok],
                                                            in1=nmr[:, :ntok], op=ALU.add),
                     reads=[tt_t[k], nmr_t], writes=[tt_t[k]])
                for (func, sc, bi, xr, dap, dtl) in outs:
                    S.op("act", lambda h, c=c, k=k, func=func, sc=sc, bi=bi, dap=dap:
                         h.activation(out=dap(c), in_=tt_[:, k, :ntok], func=func, scale=sc(c), bias=bi(c)),
                         reads=[tt_t[k], par_t] + list(xr), writes=[dtl(c)])
                if post is not None and c >= 2:
                    post(c - 2)
            if post is not None:
                for c in range(max(0, nch - 2), nch):
                    post(c)

        def ln_tmps(st, pfx, nch):
            zb = sb(pfx + "zb", [128, nch, GT], BF16, st)
            z2b = sb(pfx + "z2b", [128, nch, GT], BF16, st)
            rstd = sb(pfx + "rstd", [128, GT], F32, st)
            nmr = sb(pfx + "nmr", [128, GT], F32, st)
            nmr_t = T()
            msq, msq_t = nmr, nmr_t
            tt_ = sb(pfx + "tt", [128, 2, GT], F32, st)
            return (zb, [T() for _ in range(nch)], z2b, [T() for _ in range(nch)], rstd, T(), nmr, nmr_t,
                    msq, msq_t, tt_, [T() for _ in range(2)])

        with ExitStack() as st:
            win = sb("win", [128, 8, 1792], BF16, st)
            win_t = T()
            msk = sb("msk", [128, 3 * 512], BF16, st)
            msk_t = T()
            S.dma("pool", msk[:], masks, writes=[msk_t], sem_tile=msk_t)
            mask_own = msk[:, 0:512]
            mask_prev = msk[:, 512:1024]
            mask_first = msk[:, 1024:1536]
            S.dma("pool", win[:], w_in.rearrange("(c p) m -> p c m", p=128), writes=[win_t], sem_tile=win_t)
            diag = sb("diag", [128, 4 * 31, 128], BF16, st)
            diag_t = T()
            S.op("dve", lambda h: h.tensor_tensor(
                out=diag[:, :, :], in0=idf[:, :].unsqueeze(1).to_broadcast([128, 124, 128]),
                in1=par_wdw[:, :].unsqueeze(2).to_broadcast([128, 124, 128]), op=ALU.mult),
                reads=[par_t], writes=[diag_t])
            xb = [sb(f"xb{i}", [128, 8, GT], BF16, st) for i in range(2)]
            xb_t = [T() for _ in range(2)]
            hbuf = sb("hbuf", [128, 4, 32 + GT], BF16, st)
            hh_t = [T() for _ in range(4)]
            hb_t = [T() for _ in range(4)]
            qb = sb("qb", [128, 4, GT], BF16, st)
            qb_t = T()
            kA = sb("kA", [128, 128 + GT], BF16, st)
            kB = sb("kB", [128, 128 + GT], BF16, st)
            kh_t, kb_t = T(), T()
            vz = sb("vz", [128, 5, 2, 128], BF16, st)
            vh_t, vb_t = T(), T()
            sg = sb("sg", [128, 2, GT], F32, st)
            sg_t = [T(), T()]
            yc = sb("yc", [128, 4, GT], F32, st)
            yc_t = [T() for _ in range(4)]
            lt = ln_tmps(st, "c", 4)
            pT = sb("pT", [128, 2, 4, GT], BF16, st)
            pT_t = [[T() for _ in range(4)] for _ in range(2)]
            den = sb("den", [128, 2, 2, GT], F32, st)
            den_t = [[T(), T()], [T(), T()]]
            S.op("pool", lambda h: h.memset(kA[:], 0.0), writes=[kh_t, kb_t])
            S.op("pool", lambda h: h.memset(kB[:], 0.0), writes=[kh_t, kb_t])
            S.op("pool", lambda h: h.memset(vz[:], 0.0), writes=[vh_t, vb_t])
            S.op("pool", lambda h: h.memset(hbuf[:], 0.0), writes=hh_t + hb_t)

            def load_x(gi, buf):
                if gi < 0:
                    src = xT[:, 0:HALO]
                    dst = xb[buf][:, :, 0:HALO]
                else:
                    src = xT[:, HALO + gi * GT: HALO + (gi + 1) * GT]
                    dst = xb[buf][:, :, :]
                S.dma("pool", dst, src.rearrange("(c p) t -> p c t", p=128), writes=[xb_t[buf]],
                      sem_tile=xb_t[buf])

            def inproj(col0, buf, nt, pb, pb_t):
                for k in range(8):
                    mm(pb[:, :nt], pb_t, win[:, k, col0:col0 + 128], xb[buf][:, k, :nt],
                       [win_t, xb_t[buf]], k == 0, k == 7)

            load_x(-1, 0)
            for gi in range(-1, NG):
                buf = (gi + 1) % 2
                nt = HALO if gi < 0 else GT
                if gi + 1 < NG:
                    load_x(gi + 1, (gi + 2) % 2)
                hoff = 32 + (GT - nt)
                koff = 128 + (GT - nt)
                if gi >= 0:
                    for j in range(4):
                        if gi == 0:
                            S.op("pool", lambda h, j=j: h.tensor_scalar(hbuf[:, j, 0:32], hbuf[:, j, GT:GT + 32],
                                                                        par_flag[:, 0:1], None, op0=ALU.mult),
                                 reads=[hb_t[j], par_t], writes=[hh_t[j]])
                        else:
                            S.op("pool", lambda h, j=j: h.tensor_copy(out=hbuf[:, j, 0:32],
                                                                      in_=hbuf[:, j, GT:GT + 32]),
                                 reads=[hb_t[j]], writes=[hh_t[j]])
                    S.op("pool", lambda h: h.tensor_copy(out=kA[:, 0:128], in_=kA[:, GT:GT + 128]),
                         reads=[kb_t], writes=[kh_t])
                    S.op("pool", lambda h: h.tensor_copy(out=kB[:, 0:128], in_=kB[:, GT:GT + 128]),
                         reads=[kb_t], writes=[kh_t])
                    S.op("pool", lambda h: h.tensor_copy(out=vz[:, 0, :, :], in_=vz[:, 4, :, :]),
                         reads=[vb_t], writes=[vh_t])
                for j in range(4):
                    pa, pa_t = bank()
                    inproj(j * 128, buf, nt, pa, pa_t)
                    pg, pg_t = bank()
                    inproj((4 + j) * 128, buf, nt, pg, pg_t)
                    s = j % 2
                    S.op("act", lambda h, j=j, s=s, pg=pg, nt=nt: h.activation(
                        out=sg[:, s, :nt], in_=pg[:, :nt], func=AF.Sigmoid, bias=par_b_in[:, 4 + j:5 + j]),
                        reads=[pg_t, par_t], writes=[sg_t[s]])
                    S.op("dve", lambda h, j=j, s=s, pa=pa, nt=nt, hoff=hoff: h.scalar_tensor_tensor(
                        out=hbuf[:, j, hoff:hoff + nt], in0=pa[:, :nt], scalar=par_b_in[:, j:j + 1],
                        in1=sg[:, s, :nt], op0=ALU.add, op1=ALU.mult),
                        reads=[pa_t, sg_t[s], par_t], writes=[hb_t[j]])
                if gi >= 0:
                    for j in range(4):
                        pq, pq_t = bank()
                        inproj((8 + j) * 128, buf, nt, pq, pq_t)
                        S.op("act", lambda h, j=j, pq=pq: h.activation(
                            out=qb[:, j, :], in_=pq[:, :], func=AF.Identity, bias=par_b_in[:, 8 + j:9 + j]),
                            reads=[pq_t, par_t], writes=[qb_t])
                pk, pk_t = bank()
                inproj(12 * 128, buf, nt, pk, pk_t)
                S.op("act", lambda h, pk=pk, nt=nt, koff=koff: h.activation(
                    out=kA[0:64, koff:koff + nt], in_=pk[0:64, :nt], func=AF.Identity, bias=par_b_in[0:64, 12:13]),
                    reads=[pk_t, par_t], writes=[kb_t])
                S.op("act", lambda h, pk=pk, nt=nt, koff=koff: h.activation(
                    out=kB[64:128, koff:koff + nt], in_=pk[64:128, :nt], func=AF.Identity,
                    bias=par_b_in[64:128, 12:13]),
                    reads=[pk_t, par_t], writes=[kb_t])
                nblk = nt // 128
                for b in range(nblk):
                    slot = 1 + b + (4 - nblk)
                    pv, pv_t = bank()
                    for k in range(8):
                        mm(pv[:, 0:128], pv_t, xb[buf][:, k, b * 128:(b + 1) * 128], win[:, k, 13 * 128:14 * 128],
                           [win_t, xb_t[buf]], k == 0, k == 7)
                    S.op("dve", lambda h, pv=pv, slot=slot: h.tensor_tensor(
                        out=vz[:, slot, 0, 0:64], in0=pv[:, 0:64], in1=par_bv[:, 0:64], op=ALU.add),
                        reads=[pv_t, par_t], writes=[vb_t])
                    S.op("dve", lambda h, pv=pv, slot=slot: h.tensor_tensor(
                        out=vz[:, slot, 1, 64:128], in0=pv[:, 64:128], in1=par_bv[:, 64:128], op=ALU.add),
                        reads=[pv_t, par_t], writes=[vb_t])
                if gi < 0:
                    continue
                g = gi
                gc = slice(g * GT, (g + 1) * GT)
                def conv_chunk(j):
                    pc, pc_t = bank()
                    for tap in range(31):
                        mm(pc[:, :], pc_t, diag[:, j * 31 + tap, :], hbuf[:, j, 2 + tap:2 + tap + GT],
                           [diag_t, hh_t[j], hb_t[j]], tap == 0, tap == 30)
                    S.op("act", lambda h, j=j, pc=pc: h.activation(
                        out=yc[:, j, :], in_=pc[:, :], func=AF.Identity, bias=par_cpar[:, j:j + 1]),
                        reads=[pc_t, par_t], writes=[yc_t[j]])
                    ln_pre(lambda c: yc[:, c, :], lambda c: yc_t[c], j, lt)

                def conv_ln():
                    ln_post(lambda c: yc[:, c, :], lambda c: yc_t[c], 4, ones512,
                            [(AF.Silu, lambda c: par_cpar[:, 4 + c:5 + c], lambda c: par_cpar[:, 8 + c:9 + c], [],
                              lambda c, gc=gc: ymix[:, c, gc], lambda c, g=g: ymT[c][g])], lt)
                def scores(b):
                    bp = b % 2
                    first = (g == 0 and b == 0)
                    qv = qb[:, :, b * 128:(b + 1) * 128]
                    for kv in range(2):
                        kX = kA if kv == 0 else kB
                        for po in range(2):
                            ps_, ps_t = bank()
                            kcol = b * 128 + po * 128
                            kt = kh_t if kcol < 128 else kb_t
                            mm(ps_[:, :], ps_t, kX[:, kcol:kcol + 128], qv, [kt, qb_t], True, False)
                            mk = mask_own if po == 1 else (mask_first if first else mask_prev)
                            mm(ps_[:, :], ps_t, ident_b, mk, [cst_t, msk_t], False, True)
                            idx = kv * 2 + po
                            S.op("act", lambda h, ps_=ps_, idx=idx, bp=bp: h.activation(
                                out=pT[:, bp, idx, :], in_=ps_[:, :], func=AF.Exp, scale=0.125),
                                reads=[ps_t], writes=[pT_t[bp][idx]])

                def pv(b):
                    bp = b % 2
                    pTb = pT[:, bp]
                    po_, po_t = bank()
                    pl_, pl_t = bank()
                    n = 0
                    for kv in range(2):
                        for po in range(2):
                            idx = kv * 2 + po
                            slot = b + po
                            vt = vh_t if slot == 0 else vb_t
                            mm(po_[:, :], po_t, vz[:, slot, kv, :], pTb[:, idx, :], [vt, pT_t[bp][idx]], n == 0, n == 3)
                            n += 1
                    n = 0
                    for kv in range(2):
                        for po in range(2):
                            idx = kv * 2 + po
                            mm(pl_[:, :], pl_t, ones0 if kv == 0 else ones1, pTb[:, idx, :], [cst_t, pT_t[bp][idx]],
                               n == 0, n == 3)
                            n += 1
                    S.op("dve", lambda h, pl_=pl_, bp=bp: h.tensor_tensor(
                        out=den[:, bp, 0, :].rearrange("p (g q) -> p g q", g=4),
                        in0=pl_[:, :].rearrange("p (g q) -> p g q", g=4),
                        in1=sinkexp[:, :].unsqueeze(2).to_broadcast([128, 4, 128]), op=ALU.add),
                        reads=[pl_t, sinkexp_t], writes=[den_t[bp][0]])
                    S.op("act", lambda h, bp=bp: h.activation(out=den[:, bp, 0, :], in_=den[:, bp, 0, :], func=AF.Ln),
                         reads=[den_t[bp][0]], writes=[den_t[bp][0]])
                    S.op("act", lambda h, bp=bp: h.activation(out=den[:, bp, 1, :], in_=den[:, bp, 0, :], func=AF.Exp,
                                                              scale=-1.0),
                         reads=[den_t[bp][0]], writes=[den_t[bp][1]])
                    tc0 = g * GT + b * 128
                    S.op("dve", lambda h, po_=po_, tc0=tc0, bp=bp: h.tensor_tensor(
                        out=ymix[:, 4:8, tc0:tc0 + 128], in0=po_[:, :].rearrange("p (g q) -> p g q", g=4),
                        in1=den[:, bp, 1, :].rearrange("p (g q) -> p g q", g=4), op=ALU.mult),
                        reads=[po_t, den_t[bp][1]], writes=[ymT[4 + j][g] for j in range(4)])

                scores(0)
                for b in range(4):
                    conv_chunk(b)
                    if b + 1 < 4:
                        scores(b + 1)
                    pv(b)
                conv_ln()
            S.flush()
        if stop_after == "A1":
            return nc

        kmT = sb("kmT", [128, 8, 256], BF16)
        kmT_t = T(name="kmT")
        vm = sb("vm", [128, 2, 1024], BF16)
        vm_t = T(name="vm")
        x1d_t = [T(name=f"x1d{g}") for g in range(NG)]
        with ExitStack() as st:
            wout = sb("wout", [128, 8, D], BF16, st)
            wout_t = [T() for _ in range(4)]
            for q4 in range(4):
                S.dma("pool", wout[:, :, q4 * 256:(q4 + 1) * 256],
                      w_out[:, q4 * 256:(q4 + 1) * 256].rearrange("(c p) m -> p c m", p=128),
                      writes=[wout_t[q4]], sem_tile=wout_t[q4])
            memb = sb("memb", [128, 8, 256], BF16, st)
            memb_t = T()
            wk = sb("wk", [128, 8, 1024], BF16, st)
            wk_t = T()
            wv = sb("wv", [128, 8, 1024], BF16, st)
            wv_t = T()
            S.dma("pool", memb[:], memT.rearrange("(c p) m -> p c m", p=128), writes=[memb_t], sem_tile=memb_t)
            S.dma("pool", wk[:], w_mkv[:, 0:1024].rearrange("(c p) m -> p c m", p=128), writes=[wk_t], sem_tile=wk_t)
            S.dma("pool", wv[:], w_mkv[:, 1024:2048].rearrange("(c p) m -> p c m", p=128), writes=[wv_t],
                  sem_tile=wv_t)

            def ph0_compute():
                for c in range(8):
                    pb, pb_t = bank()
                    for k in range(8):
                        mm(pb[:, 0:256], pb_t, wk[:, k, colsl(c)], memb[:, k, :], [wk_t, memb_t], k == 0, k == 7)
                    S.op("act", lambda h, c=c, pb=pb: h.activation(out=kmT[:, c, :], in_=pb[:, 0:256], func=AF.Copy),
                         reads=[pb_t], writes=[kmT_t])
                for mc in range(2):
                    for nh in range(2):
                        pb, pb_t = bank()
                        for k in range(8):
                            mm(pb[:, :], pb_t, memb[:, k, colsl(mc)], wv[:, k, nh * 512:(nh + 1) * 512],
                               [wv_t, memb_t], k == 0, k == 7)
                        S.op("dve", lambda h, mc=mc, nh=nh, pb=pb: h.tensor_copy(
                            out=vm[:, mc, nh * 512:(nh + 1) * 512], in_=pb[:, :]),
                            reads=[pb_t], writes=[vm_t])


            xs = [sb(f"xs{i}", [128, 8, GT], F32, st) for i in range(3)]
            xs_l = [T() for _ in range(3)]
            xs_t = [[T() for _ in range(8)] for _ in range(3)]
            lt = ln_tmps(st, "l1", 8)
            zb1 = sb("l1zb1", [128, 8, GT], BF16, st)
            z2b1 = sb("l1z2b1", [128, 8, GT], BF16, st)
            ltp = [lt, (zb1, [T() for _ in range(8)], z2b1, [T() for _ in range(8)]) + tuple(lt[4:])]

            def a2_stage1(g):
                i = g % 3
                gc = slice(g * GT, (g + 1) * GT)
                S.dma("sp", xs[i][:], xT[:, HALO + g * GT:HALO + (g + 1) * GT].rearrange("(c p) t -> p c t", p=128),
                      writes=xs_t[i], sem_tile=xs_l[i])
                for c in range(8):
                    pb, pb_t = bank()
                    for k in range(8):
                        mm(pb[:, :], pb_t, wout[:, k, colsl(c)], ymix[:, k, gc], [wout_t[c // 2], ymT[k][g]], k == 0, k == 7)
                    S.op("dve", lambda h, c=c, pb=pb, i=i: h.scalar_tensor_tensor(
                        out=xs[i][:, c, :], in0=xs[i][:, c, :], scalar=ALPHA, in1=pb[:, :],
                        op0=ALU.mult, op1=ALU.add),
                        reads=[pb_t, xs_t[i][c]], writes=[xs_t[i][c]])
                    ln_pre(lambda c, i=i: xs[i][:, c, :], lambda c, i=i: xs_t[i][c], c, ltp[g % 2], cast_eng="act")

            a2_stage1(0)
            for g in range(NG):
                i = g % 3
                gc = slice(g * GT, (g + 1) * GT)
                if g + 1 < NG:
                    a2_stage1(g + 1)

                def post1(c, i=i, gc=gc, g=g):
                    S.op("dve", lambda h: h.tensor_copy(out=ymix[:, c, gc], in_=xs[i][:, c, :]),
                         reads=[xs_t[i][c]], writes=[ymT[c][g]])

                ln_post(lambda c, i=i: xs[i][:, c, :], lambda c, i=i: xs_t[i][c], 8, ones1024,
                        [(AF.Identity, lambda c: par_lnp[:, c:c + 1], lambda c: par_lnp[:, 8 + c:9 + c], [],
                          lambda c, i=i: xs[i][:, c, :], lambda c, i=i: xs_t[i][c])], ltp[g % 2], post=post1)
                S.dma("sp", x1T[:, gc].rearrange("(c p) t -> p c t", p=128), xs[i][:],
                      reads=xs_t[i], writes=[x1d_t[g]], sem_tile=xs_l[i])
            ph0_compute()
            S.flush()
        if stop_after == "A2":
            return nc

        acc = sb("acc", [128, 8, TOK], F32)
        accT = [[T(name=f"acc{c}_{g}") for g in range(NG)] for c in range(8)]

        with ExitStack() as st:
            wmq = sb("wmq", [128, 8, D], BF16, st)
            wmq_t = [T() for _ in range(4)]
            wmo = sb("wmo", [128, 8, D], BF16, st)
            wmo_t = [T() for _ in range(4)]
            for wdst, wsrc, wts in ((wmq, w_mq, wmq_t), (wmo, w_mo, wmo_t)):
                for q4 in range(4):
                    S.dma("pool", wdst[:, :, q4 * 256:(q4 + 1) * 256],
                          wsrc[:, q4 * 256:(q4 + 1) * 256].rearrange("(c p) m -> p c m", p=128),
                          writes=[wts[q4]], sem_tile=wts[q4])
            xs1 = sb("xsb", [128, 8, GT], F32, st)
            xs1_l = T()
            xs1_t = [T() for _ in range(8)]
            qm = sb("qm", [128, 8, GT], BF16, st)
            qm_t = [T() for _ in range(8)]
            om, om_t = qm, qm_t
            pmb = sb("pmb", [128, 1, 2, GT], BF16, st)
            pm_t = [[T(), T()]]
            rl = sb("rl", [128, 1, GT], F32, st)
            rl_t = [T()]
            lt = ln_tmps(st, "l2", 8)
            zb1 = sb("l2zb1", [128, 8, GT], BF16, st)
            z2b1 = sb("l2z2b1", [128, 8, GT], BF16, st)
            ltp = [lt, (zb1, [T() for _ in range(8)], z2b1, [T() for _ in range(8)]) + tuple(lt[4:])]

            def b_stage1(g):
                i = g % 2
                gc = slice(g * GT, (g + 1) * GT)
                S.dma("sp", xs1[:], x1T[:, gc].rearrange("(c p) t -> p c t", p=128),
                      reads=[x1d_t[g]], writes=xs1_t, sem_tile=xs1_l)
                for c in range(8):
                    pb, pb_t = bank()
                    for k in range(8):
                        mm(pb[:, :], pb_t, wmq[:, k, colsl(c)], ymix[:, k, gc], [wmq_t[c // 2], ymT[k][g]], k == 0, k == 7)
                    S.op("act", lambda h, c=c, pb=pb: h.activation(out=qm[:, c, :], in_=pb[:, :], func=AF.Copy),
                         reads=[pb_t], writes=[qm_t[c]])
                for hd in range(4):
                    par = 0
                    for mc in range(2):
                        ps_, ps_t = bank()
                        for dc in range(2):
                            mm(ps_[:, :], ps_t, kmT[:, 2 * hd + dc, colsl(mc)], qm[:, 2 * hd + dc, :],
                               [kmT_t, qm_t[2 * hd + dc]], dc == 0, dc == 1)
                        S.op("act", lambda h, ps_=ps_, par=par, mc=mc: h.activation(
                            out=pmb[:, par, mc, :], in_=ps_[:, :], func=AF.Exp, scale=1.0 / 16.0),
                            reads=[ps_t], writes=[pm_t[par][mc]])
                    pl_, pl_t = bank()
                    for mc in range(2):
                        mm(pl_[:, :], pl_t, onesP, pmb[:, par, mc, :], [cst_t, pm_t[par][mc]],
                           mc == 0, mc == 1)
                    S.op("act", lambda h, pl_=pl_, par=par: h.activation(out=rl[:, par, :], in_=pl_[:, :], func=AF.Ln),
                         reads=[pl_t], writes=[rl_t[par]])
                    S.op("act", lambda h, par=par: h.activation(out=rl[:, par, :], in_=rl[:, par, :], func=AF.Exp,
                                                                scale=-1.0),
                         reads=[rl_t[par]], writes=[rl_t[par]])
                    for dc in range(2):
                        po_, po_t = bank()
                        for mc in range(2):
                            mm(po_[:, :], po_t, vm[:, mc, colsl(2 * hd + dc)], pmb[:, par, mc, :],
                               [vm_t, pm_t[par][mc]], mc == 0, mc == 1)
                        S.op("dve", lambda h, po_=po_, par=par, hd=hd, dc=dc: h.tensor_tensor(
                            out=om[:, 2 * hd + dc, :], in0=po_[:, :], in1=rl[:, par, :], op=ALU.mult),
                            reads=[po_t, rl_t[par]], writes=[om_t[2 * hd + dc]])
                for c in range(8):
                    pb, pb_t = bank()
                    for k in range(8):
                        mm(pb[:, :], pb_t, wmo[:, k, colsl(c)], om[:, k, :], [wmo_t[c // 2], om_t[k]], k == 0, k == 7)
                    S.op("dve", lambda h, c=c, pb=pb, gc=gc: h.scalar_tensor_tensor(
                        out=acc[:, c, gc], in0=xs1[:, c, :], scalar=ALPHA, in1=pb[:, :],
                        op0=ALU.mult, op1=ALU.add),
                        reads=[pb_t, xs1_t[c]], writes=[accT[c][g]])
                    ln_pre(lambda c, gc=gc: acc[:, c, gc], lambda c, g=g: accT[c][g], c, ltp[i], cast_eng="act")

            b_stage1(0)
            for g in range(NG):
                i = g % 2
                gc = slice(g * GT, (g + 1) * GT)
                if g + 1 < NG:
                    b_stage1(g + 1)
                def post2(c, gc=gc, g=g):
                    S.op("dve", lambda h: h.tensor_scalar(ymix[:, c, gc], acc[:, c, gc], 1.0 / ALPHA, None, op0=ALU.mult),
                         reads=[accT[c][g]], writes=[ymT[c][g]])

                ln_post(lambda c, gc=gc: acc[:, c, gc], lambda c, g=g: accT[c][g], 8, ones1024,
                        [(AF.Identity, lambda c: par_lnpa[:, c:c + 1], lambda c: par_lnpa[:, 8 + c:9 + c], [lnpa_t],
                          lambda c, gc=gc: acc[:, c, gc], lambda c, g=g: accT[c][g])], ltp[i], post=post2)
            S.flush()
        if stop_after == "B":
            S.dma("sp", outT.rearrange("(c p) t -> p c t", p=128), acc[:],
                  reads=[x for row in accT for x in row], writes=[], sem_tile=T())
            S.flush()
            return nc

        with ExitStack() as st:
            wrt = sb("wrt", [128, 8, 20], F32, st)
            wrt_t = T()
            S.dma("sp", wrt[:], w_rt.rearrange("p (c n) -> p c n", c=8), writes=[wrt_t], sem_tile=wrt_t)
            selt = sb("selt", [16, 16, 128], F32, st)
            selt_t = T()
            S.dma("sp", selt[:], sel.rearrange("k (e m) -> k e m", e=16), writes=[selt_t], sem_tile=selt_t)
            wg = [sb(f"wg{i}", [128, 8, 512], BF16, st) for i in range(2)]
            wu = [sb(f"wu{i}", [128, 8, 512], BF16, st) for i in range(2)]
            wd = [sb(f"wd{i}", [128, 4, D], BF16, st) for i in range(2)]
            wg_t = [T() for _ in range(2)]
            wu_t = [T() for _ in range(2)]
            wd_t = [T() for _ in range(2)]
            cbc = sb("cbc", [128, 2, GT], F32, st)
            cbc_t = [T(), T()]
            sgm = sb("sgm", [128, 2, GT], F32, st)
            sgm_t = [T(), T()]
            tm = sb("tm", [128, 2, GT], F32, st)
            tm_t = [T(), T()]
            hh = sb("hh", [128, 2, 4, GT], BF16, st)
            hh_t2 = [[T() for _ in range(4)] for _ in range(2)]

            def load_expert(e):
                i = e % 2
                S.dma("pool", wg[i][:], w_gate[e].rearrange("(c p) f -> p c f", p=128), writes=[wg_t[i]],
                      sem_tile=wg_t[i])
                S.dma("pool", wu[i][:], w_up[e].rearrange("(c p) f -> p c f", p=128), writes=[wu_t[i]],
                      sem_tile=wu_t[i])
                S.dma("pool", wd[i][:], w_down[e].rearrange("(c p) f -> p c f", p=128), writes=[wd_t[i]],
                      sem_tile=wd_t[i])

            load_expert(0)
            lg = sb("lg", [128, 16, 20], F32, st)
            lg_t = T()
            lgT = wg[1][:].bitcast(F32).rearrange("p c f -> p (c f)")
            for g in range(NG):
                gc = slice(g * GT, (g + 1) * GT)
                pb, pb_t = bank()
                for k in range(8):
                    mm(pb[0:20, :], pb_t, wrt[:, k, :], acc[:, k, gc], [accT[k][g], wrt_t], k == 0, k == 7)
                S.op("act", lambda h, pb=pb, gc=gc: h.activation(
                    out=lgT[0:20, gc], in_=pb[0:20, :], func=AF.Identity, scale=1.0 / ALPHA, bias=par_brc[0:20, 0:1]),
                    reads=[pb_t, par_t], writes=[wg_t[1]])
            for g in range(NG):
                pb, pb_t = bank()
                for q in range(4):
                    tt = g * 4 + q
                    S.op("pe", lambda h, pb=pb, q=q, tt=tt: h.transpose(
                        out=pb[:, q * 20:(q + 1) * 20], in_=lgT[0:20, tt * 128:(tt + 1) * 128],
                        identity=idf[0:20, 0:20]),
                        reads=[wg_t[1], par_t], writes=[pb_t], inc=True)
                S.op("dve", lambda h, pb=pb, g=g: h.tensor_copy(
                    out=lg[:, 4 * g:4 * g + 4, :].rearrange("p a b -> p (a b)"), in_=pb[:, 0:80]),
                    reads=[pb_t], writes=[lg_t])
            R = sb("R", [128, 16, 64], F32, st)
            R_t = T()

            def rop(eng, fn):
                S.op(eng, fn, reads=[lg_t, R_t], writes=[R_t])

            def b3(ap2, n):
                return ap2.unsqueeze(2).to_broadcast([128, 16, n])

            gl = lg[:, :, 0:4]
            gmax, gsum, gp = R[:, :, 0], R[:, :, 1], R[:, :, 2]
            gm = R[:, :, 4:8]
            gd = R[:, :, 8:12]
            el = R[:, :, 12:16]
            tmp = R[:, :, 16:20]
            m1, m2, d21, s1, s2 = R[:, :, 20], R[:, :, 21], R[:, :, 22], R[:, :, 23], R[:, :, 24]
            mk1 = R[:, :, 28:32]
            el2 = R[:, :, 32:36]
            mk2 = R[:, :, 36:40]
            cg = R[:, :, 40:44]
            comb = R[:, :, 48:64]
            rop("dve", lambda h: h.tensor_reduce(out=gmax, in_=gl, axis=AX.X, op=ALU.max))
            rop("dve", lambda h: h.tensor_tensor(out=gm, in0=gl, in1=b3(gmax, 4), op=ALU.is_equal))
            rop("dve", lambda h: h.tensor_tensor(out=gd, in0=gl, in1=b3(gmax, 4), op=ALU.subtract))
            rop("act", lambda h: h.activation(out=gd, in_=gd, func=AF.Exp))
            rop("dve", lambda h: h.tensor_reduce(out=gsum, in_=gd, axis=AX.X, op=ALU.add))
            rop("dve", lambda h: h.reciprocal(out=gp, in_=gsum))
            for gg in range(4):
                dst = el if gg == 0 else tmp
                rop("dve", lambda h, gg=gg, dst=dst: h.tensor_tensor(
                    out=dst, in0=lg[:, :, 4 + 4 * gg:8 + 4 * gg], in1=b3(R[:, :, 4 + gg], 4), op=ALU.mult))
                if gg > 0:
                    rop("dve", lambda h: h.tensor_tensor(out=el, in0=el, in1=tmp, op=ALU.add))
            rop("dve", lambda h: h.tensor_reduce(out=m1, in_=el, axis=AX.X, op=ALU.max))
            rop("dve", lambda h: h.tensor_tensor(out=mk1, in0=el, in1=b3(m1, 4), op=ALU.is_equal))
            rop("dve", lambda h: h.scalar_tensor_tensor(out=el2, in0=mk1, scalar=-1.0e30, in1=el,
                                                        op0=ALU.mult, op1=ALU.add))
            rop("dve", lambda h: h.tensor_reduce(out=m2, in_=el2, axis=AX.X, op=ALU.max))
            rop("dve", lambda h: h.tensor_tensor(out=mk2, in0=el2, in1=b3(m2, 4), op=ALU.is_equal))
            rop("dve", lambda h: h.tensor_tensor(out=d21, in0=m2, in1=m1, op=ALU.subtract))
            rop("act", lambda h: h.activation(out=d21, in_=d21, func=AF.Exp))
            rop("dve", lambda h: h.tensor_scalar(s1, d21, 1.0, None, op0=ALU.add))
            rop("dve", lambda h: h.reciprocal(out=s1, in_=s1))
            rop("dve", lambda h: h.tensor_tensor(out=s2, in0=d21, in1=s1, op=ALU.mult))
            rop("dve", lambda h: h.tensor_tensor(out=s1, in0=s1, in1=gp, op=ALU.mult))
            rop("dve", lambda h: h.tensor_tensor(out=s2, in0=s2, in1=gp, op=ALU.mult))
            rop("dve", lambda h: h.tensor_tensor(out=cg, in0=mk1, in1=b3(s1, 4), op=ALU.mult))
            rop("dve", lambda h: h.tensor_tensor(out=tmp, in0=mk2, in1=b3(s2, 4), op=ALU.mult))
            rop("dve", lambda h: h.tensor_tensor(out=cg, in0=cg, in1=tmp, op=ALU.add))
            for gg in range(4):
                rop("dve", lambda h, gg=gg: h.tensor_tensor(
                    out=R[:, :, 48 + 4 * gg:52 + 4 * gg], in0=cg, in1=b3(R[:, :, 4 + gg], 4), op=ALU.mult))
            combT = sb("combT", [16, TOK], F32, st)
            combT_t = [T() for _ in range(NG)]
            for g in range(NG):
                pb, pb_t = bank()
                for q in range(4):
                    tt = g * 4 + q
                    S.op("pe", lambda h, pb=pb, q=q, tt=tt: h.transpose(
                        out=pb[0:16, q * 128:(q + 1) * 128], in_=R[:, tt, 48:64], identity=idf[:]),
                        reads=[R_t, par_t], writes=[pb_t], inc=True)
                S.op("act", lambda h, pb=pb, g=g: h.activation(
                    out=combT[:, g * GT:(g + 1) * GT], in_=pb[0:16, :], func=AF.Copy),
                    reads=[pb_t], writes=[combT_t[g]])
            ob = sb("ob", [128, 2, 2, GT], F32, st)
            ob_l = [T(), T()]
            ob_t = [T(), T()]
            wdv = wd[0][:].bitcast(F32)
            l3n_t = T()
            lt3 = (wg[0], [T() for _ in range(8)], wu[0], [T() for _ in range(8)], wdv[:, 0, :], T(),
                   wdv[:, 1, :], l3n_t, wdv[:, 1, :], l3n_t, wdv[:, 2:4, :], [T(), T()])
            ln3_state = {"fenced": False}

            def ln3(g):
                gc = slice(g * GT, (g + 1) * GT)
                if not ln3_state["fenced"]:
                    ln3_state["fenced"] = True
                    allt = [wg_t[0], wu_t[0], wd_t[0]] + lt3[1] + lt3[3] + [lt3[5], lt3[7]] + lt3[11]
                    S.op("pool", lambda h: h.memset(wdv[:, 0, 0:2], 0.0), writes=allt)

                def post(c):
                    if c % 2 == 1:
                        bf = (c // 2) % 2
                        S.dma("sp", outT[(c - 1) * 128:(c + 1) * 128, gc].rearrange("(c p) t -> p c t", p=128),
                              ob[:, bf, :, :], reads=[ob_t[bf]], writes=[], sem_tile=ob_l[bf])

                layernorm(lambda c, gc=gc: acc[:, c, gc], lambda c, g=g: accT[c][g], 8, ones1024,
                          [(AF.Identity, lambda c: par_lnp[:, 32 + c:33 + c], lambda c: par_lnp[:, 40 + c:41 + c], [],
                            lambda c: ob[:, (c // 2) % 2, c % 2, :], lambda c: ob_t[(c // 2) % 2])], lt3, post=post)

            def gateup(e, g, ci):
                i = e % 2
                gc = slice(g * GT, (g + 1) * GT)
                pcb, pcb_t = bank()
                mm(pcb[:, :], pcb_t, selt[:, e, :], combT[:, gc], [selt_t, combT_t[g]], True, True)
                S.op("act", lambda h, pcb=pcb, ci=ci: h.activation(out=cbc[:, ci, :], in_=pcb[:, :], func=AF.Copy),
                     reads=[pcb_t], writes=[cbc_t[ci]])
                for fc in range(4):
                    s_ = fc % 2
                    pg_, pg_t = bank()
                    for k in range(8):
                        mm(pg_[:, :], pg_t, wg[i][:, k, colsl(fc)], ymix[:, k, gc], [wg_t[i], ymT[k][g]],
                           k == 0, k == 7)
                    pu_, pu_t = bank()
                    for k in range(8):
                        mm(pu_[:, :], pu_t, wu[i][:, k, colsl(fc)], ymix[:, k, gc], [wu_t[i], ymT[k][g]],
                           k == 0, k == 7)
                    S.op("act", lambda h, pg_=pg_, s_=s_: h.activation(out=sgm[:, s_, :], in_=pg_[:, :], func=AF.Silu),
                         reads=[pg_t], writes=[sgm_t[s_]])
                    S.op("dve", lambda h, pu_=pu_, s_=s_: h.tensor_tensor(
                        out=tm[:, s_, :], in0=sgm[:, s_, :], in1=pu_[:, :], op=ALU.mult),
                        reads=[pu_t, sgm_t[s_]], writes=[tm_t[s_]])
                    S.op("pool", lambda h, s_=s_, ci=ci, fc=fc: h.tensor_tensor(
                        out=hh[:, ci, fc, :], in0=tm[:, s_, :], in1=cbc[:, ci, :], op=ALU.mult),
                        reads=[tm_t[s_], cbc_t[ci]], writes=[hh_t2[ci][fc]])

            def down(e, g, ci):
                i = e % 2
                gc = slice(g * GT, (g + 1) * GT)
                for dc in range(8):
                    pd_, pd_t = bank()
                    for fc in range(4):
                        mm(pd_[:, :], pd_t, wd[i][:, fc, colsl(dc)], hh[:, ci, fc, :], [wd_t[i], hh_t2[ci][fc]],
                           fc == 0, fc == 3)
                    S.op("dve", lambda h, pd_=pd_, dc=dc, gc=gc: h.tensor_tensor(
                        out=acc[:, dc, gc], in0=acc[:, dc, gc], in1=pd_[:, :], op=ALU.add),
                        reads=[pd_t, accT[dc][g]], writes=[accT[dc][g]])
                if e == 15 and g >= 1:
                    ln3(g - 1)

            units = [(e, g) for e in range(16) for g in range(NG)]
            for u, (e, g) in enumerate(units):
                gateup(e, g, u % 2)
                if u >= 1:
                    pe_, pg2 = units[u - 1]
                    down(pe_, pg2, (u - 1) % 2)
                if g == 0 and e + 1 < 16:
                    load_expert(e + 1)
            down(15, NG - 1, (len(units) - 1) % 2)
            ln3(NG - 1)
            S.flush()
    return nc


def _chunked(v, n):
    return np.ascontiguousarray(np.asarray(v, np.float32).reshape(n, 128).T)


def prepare_inputs(inp):
    f = lambda k: np.asarray(inp[k], np.float32)
    x, mem = f("x"), f("mem")
    w_in, b_in = f("w_in")[0], f("b_in")[0]
    qperm = np.empty(512, np.int64)
    for j in range(4):
        for r in range(2):
            qperm[j * 128 + r * 64:(j * 128 + r * 64 + 64)] = (r * 4 + j) * 64 + np.arange(64)
    cols = np.concatenate([np.arange(1024), 1024 + qperm, np.arange(1536, 1792)])
    w_in_p = np.ascontiguousarray(w_in[:, cols])
    b_in_p = b_in[cols]
    shared = {
        "w_in": w_in_p,
        "b_in": _chunked(b_in_p, 14),
        "bv_rep": np.ascontiguousarray(np.tile(b_in_p[1664:1792][None, :], (128, 1))),
        "wdw": np.ascontiguousarray(f("w_dw")[0].reshape(31, 4, 128).transpose(2, 1, 0).reshape(128, 124)),
        "cpar": np.concatenate([_chunked(f("b_dw")[0], 4), _chunked(f("g_conv_norm")[0], 4),
                                _chunked(f("b_conv_norm")[0], 4)], axis=1),
        "w_out": np.ascontiguousarray(f("w_out")[0][np.concatenate([np.arange(512), 512 + qperm])]),
        "lnp": np.concatenate([_chunked(f(k)[0], 8) for k in ("g_ln1", "b_ln1", "g_ln2", "b_ln2", "g_ln3", "b_ln3")],
                              axis=1),
        "w_mq": f("w_mq")[0], "w_mkv": f("w_mkv")[0], "w_mo": f("w_mo")[0],
        "w_gate": f("w_gate")[0], "w_up": f("w_up")[0], "w_down": f("w_down")[0],
    }
    sk = f("attn_sinks")[0]
    shared["sinks"] = np.ascontiguousarray(np.concatenate([np.tile(sk[None, 0:4], (64, 1)),
                                                           np.tile(sk[None, 4:8], (64, 1))], axis=0))
    wr = np.concatenate([f("w_group")[0]] + [f("w_router")[0][g] for g in range(4)], axis=1)
    shared["w_rt"] = np.ascontiguousarray(wr.reshape(8, 128, 20).transpose(1, 0, 2).reshape(128, 160))
    br = np.concatenate([f("b_group")[0], f("b_router")[0].reshape(-1)])
    shared["b_rt"] = np.ascontiguousarray(np.tile(br[None, :], (128, 1)))
    shared["b_rt_col"] = np.ascontiguousarray(br.reshape(20, 1))
    kk = np.arange(128)[:, None]
    qq = np.arange(128)[None, :]
    m_own = np.where(kk <= qq, 0.0, NEG).astype(np.float32)
    m_prev = np.where(kk > qq, 0.0, NEG).astype(np.float32)
    m_none = np.full((128, 128), NEG, np.float32)
    eye = np.eye(128, dtype=np.float32)
    o0 = np.zeros((128, 128), np.float32); o0[:, :64] = 1.0
    o1 = np.zeros((128, 128), np.float32); o1[:, 64:] = 1.0
    on = np.ones((128, 128), np.float32)
    shared["consts"] = np.ascontiguousarray(np.concatenate([eye, o0, o1, on / 512.0, on / 1024.0, on], axis=1))
    shared["identf"] = eye
    sel = np.zeros((16, 16, 128), np.float32)
    for e in range(16):
        sel[e, e, :] = 1.0
    shared["sel"] = sel.reshape(16, 2048)
    in_maps = []
    for c in range(NCORES):
        b, s0 = c // 4, (c % 4) * TOK
        halo = x[b, s0 - HALO:s0] if s0 > 0 else np.zeros((HALO, D), np.float32)
        m = dict(shared)
        m["xT"] = np.ascontiguousarray(np.concatenate([halo, x[b, s0:s0 + TOK]], axis=0).T)
        m["flag"] = np.full((128, 1), 1.0 if s0 > 0 else 0.0, np.float32)
        m["memT"] = np.ascontiguousarray(mem[b].T)
        mf = m_prev if s0 > 0 else m_none
        m["masks"] = np.ascontiguousarray(np.concatenate([np.tile(m_own, (1, 4)), np.tile(m_prev, (1, 4)),
                                                          np.tile(mf, (1, 4))], axis=1))
        in_maps.append(m)
    return in_maps


_NC_CACHE = {}


def kernel(**inputs):
    in_maps = prepare_inputs(inputs)
    if "nc" not in _NC_CACHE:
        _NC_CACHE["nc"] = build_program()
    nc = _NC_CACHE["nc"]
    res = run_bass_kernel_spmd(nc, in_maps, core_ids=list(range(NCORES)))
    out = np.empty((2, SEQ, D), np.float32)
    for c in range(NCORES):
        b, s0 = c // 4, (c % 4) * TOK
        out[b, s0:s0 + TOK, :] = np.asarray(res.results[c]["outT"], np.float32).T
    return out
```

```python
import numpy as np
from contextlib import ExitStack
import concourse.bass as bass
import concourse.mybir as mybir
from concourse.bass_utils import run_bass_kernel_spmd

F32, BF16 = mybir.dt.float32, mybir.dt.bfloat16
AF = mybir.ActivationFunctionType
ALU = mybir.AluOpType
AX = mybir.AxisListType

NCORES = 8
D = 1024
SEQ = 8192
TOK = 2048
NG = 4
GT = 512
HALO = 128
XW = HALO + TOK
ALPHA = 2.0 ** 0.25
EPS = 1e-5
NEG = -30000.0
NSEM = 48
DEBUG = False


class T:
    __slots__ = ("ap", "w", "r", "dsem", "dcnt", "name", "wsame")

    def __init__(self, ap=None, name=""):
        self.ap = ap
        self.w = None
        self.r = {}
        self.dsem = None
        self.dcnt = 0
        self.name = name


class Eng:
    def __init__(self, name, sem):
        self.name = name
        self.sem = sem
        self.count = 0
        self.observed = {}
        self.ops = []


class Sched:
    def __init__(self, nc, stack):
        self.nc = nc
        self.stack = stack
        self.semn = 0
        self.sem_pool = [self.stack.enter_context(self.nc.semaphore(f"s{i}")) for i in range(NSEM)]
        self.engs = {}
        for n in ("pe", "act", "dve", "pool", "sp"):
            self.engs[n] = Eng(n, self.newsem("e_" + n))
        self.pending_dma = {}
        pool = list(self.sem_pool)
        with self.nc.Block() as block:
            @block.sync
            def _(h):
                for sm in pool:
                    h.sem_clear(sm)

    def newsem(self, name):
        self.semn += 1
        return self.sem_pool[self.semn - 1]

    def _collect(self, e, reads, writes):
        deps = {}

        def add(tok, raw):
            if tok is None:
                return
            sem, val = tok
            if sem is e.sem:
                if e.name == "pe":
                    return
            k = sem.name if hasattr(sem, "name") else id(sem)
            k = id(sem)
            if e.observed.get(k, 0) >= val:
                return
            if k not in deps or deps[k][1] < val:
                deps[k] = (sem, val)

        for t in reads:
            add(t.w, True)
        for t in writes:
            add(t.w, False)
            for tok in t.r.values():
                add(tok, False)
        waits = list(deps.values())
        for sem, val in waits:
            e.observed[id(sem)] = val
        return waits

    def op(self, eng, fn, reads=(), writes=(), inc=True, attach=True):
        e = self.engs[eng]
        waits = self._collect(e, reads, writes)
        if inc:
            e.count += 1
            tok = (e.sem, e.count)
        else:
            tok = (e.sem, e.count + 1)
        for t in reads:
            t.r[id(e.sem)] = tok
        for t in writes:
            t.w = tok
            t.r = {}
        esem = e.sem
        can_attach = attach and eng in ("act", "dve", "pool")

        def emit(h):
            ws = list(waits)
            last = ws.pop() if (ws and can_attach) else None
            for sem, val in ws:
                h.wait_ge(sem, val)
            ins = fn(h)
            if last is not None:
                ins._wait_ge(last[0], last[1])
            if inc:
                ins.then_inc(esem, 1)

        e.ops.append(emit)

    def dma(self, queue, out_ap, in_ap, reads=(), writes=(), sem_tile=None, **kw):
        e = self.engs[queue]
        waits = self._collect(e, reads, writes)
        st = sem_tile
        if st.dsem is None:
            st.dsem = self.newsem("d")
        st.dcnt += 1
        tok = (st.dsem, 16 * st.dcnt)
        for t in reads:
            t.r[id(st.dsem)] = tok
        for t in writes:
            t.w = tok
            t.r = {}
        self.pending_dma[id(st.dsem)] = tok
        dsem = st.dsem

        def emit(h):
            for sem, val in waits:
                h.wait_ge(sem, val)
            h.dma_start(out=out_ap, in_=in_ap, **kw).then_inc(dsem, 16)

        e.ops.append(emit)

    def wait_all_dma(self, eng="sp"):
        e = self.engs[eng]
        toks = list(self.pending_dma.values())
        self.pending_dma = {}

        def emit(h):
            for sem, val in toks:
                h.wait_ge(sem, val)

        e.ops.append(emit)

    def flush(self):
        self.wait_all_dma("sp")
        with self.nc.Block() as block:
            for name, deco in (("pool", block.gpsimd), ("pe", block.tensor), ("act", block.scalar),
                               ("dve", block.vector), ("sp", block.sync)):
                ops = self.engs[name].ops

                def body(h, ops=ops):
                    for f in ops:
                        f(h)

                deco(body)
                self.engs[name].ops = []


def build_program(stop_after=None):
    nc = bass.Bass("TRN2", target_bir_lowering=False)
    dt = nc.dram_tensor

    def din(name, shape, dtype=F32):
        return dt(name, list(shape), dtype, kind="ExternalInput").ap()

    xT = din("xT", [D, XW])
    flag = din("flag", [128, 1])
    memT = din("memT", [D, 256])
    w_in = din("w_in", [D, 1792])
    b_in = din("b_in", [128, 14])
    bv_rep = din("bv_rep", [128, 128])
    wdw = din("wdw", [128, 4 * 31])
    cpar = din("cpar", [128, 12])
    sinks = din("sinks", [128, 4])
    w_out = din("w_out", [D, D])
    lnp = din("lnp", [128, 48])
    w_mq = din("w_mq", [D, D])
    w_mkv = din("w_mkv", [D, 2 * D])
    w_mo = din("w_mo", [D, D])
    w_rt = din("w_rt", [128, 8 * 20])
    b_rt = din("b_rt", [128, 20])
    b_rt_col = din("b_rt_col", [20, 1])
    w_gate = din("w_gate", [16, D, 512])
    w_up = din("w_up", [16, D, 512])
    w_down = din("w_down", [16, 512, D])
    masks = din("masks", [128, 3 * 512])
    consts = din("consts", [128, 6 * 128])
    identf = din("identf", [128, 128])
    sel = din("sel", [16, 16 * 128])
    outT = dt("outT", [D, TOK], F32, kind="ExternalOutput").ap()
    x1T = dt("x1T", [D, TOK], F32, kind="ExternalOutput" if DEBUG else "Internal").ap()

    with ExitStack() as stack:
        S = Sched(nc, stack)
        ec = stack.enter_context

        def sb(name, shape, dtype, st=None):
            return (st or stack).enter_context(nc.sbuf_tensor(name, list(shape), dtype))

        banks = []
        for i in range(8):
            p = ec(nc.psum_tensor(f"ps{i}", [128, 512], F32))
            banks.append((p, T(name=f"ps{i}")))
        bstate = {"i": 0}

        def bank():
            b = banks[bstate["i"] % 8]
            bstate["i"] += 1
            return b

        def mm(out_ap, outT_, lhsT, rhs, reads, start, stop):
            S.op("pe", lambda h: h.matmul(out_ap, lhsT=lhsT, rhs=rhs, start=start, stop=stop),
                 reads=reads, writes=[outT_], inc=stop)

        ymix = sb("ymix", [128, 8, TOK], BF16)
        ymT = [[T(name=f"ym{c}_{g}") for g in range(NG)] for c in range(8)]
        par_b_in = sb("p_b_in", [128, 14], F32)
        par_cpar = sb("p_cpar", [128, 12], F32)
        par_sinks = sb("p_sinks", [128, 4], F32)
        par_lnp = sb("p_lnp", [128, 48], F32)
        par_lnpa = sb("p_lnpa", [128, 16], F32)
        par_flag = sb("p_flag", [128, 1], F32)
        par_bv = sb("p_bv", [128, 128], F32)
        par_wdw = sb("p_wdw", [128, 4 * 31], F32)
        par_brt = sb("p_brt", [128, 20], F32)
        par_brc = sb("p_brc", [20, 1], F32)
        cst = sb("cst", [128, 6 * 128], BF16)
        idf = sb("idf", [128, 128], F32)
        par_t = T(name="params")
        ident_b = cst[:, 0:128]
        ones0 = cst[:, 128:256]
        ones1 = cst[:, 256:384]
        ones512 = cst[:, 384:512]
        ones1024 = cst[:, 512:640]
        onesP = cst[:, 640:768]

        plist = [(par_b_in, b_in, "sp"), (par_cpar, cpar, "sp"), (par_sinks, sinks, "sp"), (par_lnp, lnp, "sp"),
                 (par_flag, flag, "sp"), (par_bv, bv_rep, "sp"), (par_wdw, wdw, "sp"), (par_brt, b_rt, "sp"), (par_brc, b_rt_col, "sp"),
                 (idf, identf, "sp"), (cst, consts, "pool")]
        cst_t = T(name="consts")
        for dst, src, q in plist:
            S.dma(q, dst[:], src, writes=[], sem_tile=(par_t if q == "sp" else cst_t))
        par_t.w = (par_t.dsem, 16 * par_t.dcnt)
        cst_t.w = (cst_t.dsem, 16 * cst_t.dcnt)
        sinkexp = sb("sinkexp", [128, 4], F32)
        sinkexp_t = T(name="sinkexp")
        S.op("act", lambda h: h.activation(out=sinkexp[:], in_=par_sinks[:], func=AF.Exp),
             reads=[par_t], writes=[sinkexp_t])
        lnpa_t = T(name="lnpa")
        S.op("dve", lambda h: h.tensor_scalar(par_lnpa[:], par_lnp[:, 16:32], ALPHA, None, op0=ALU.mult),
             reads=[par_t], writes=[lnpa_t])

        def colsl(c):
            return slice(c * 128, (c + 1) * 128)

        def layernorm(z_ap, z_t, nch, ones_ap, outs, tmp, ntok=GT, post=None):
            for c in range(nch):
                ln_pre(z_ap, z_t, c, tmp, ntok)
            ln_post(z_ap, z_t, nch, ones_ap, outs, tmp, ntok, post)

        def ln_pre(z_ap, z_t, c, tmp, ntok=GT, cast_eng="dve"):
            zb, zb_t, z2b, z2b_t = tmp[0:4]
            if cast_eng == "act":
                S.op("act", lambda h, c=c: h.activation(out=zb[:, c, :ntok], in_=z_ap(c), func=AF.Copy),
                     reads=[z_t(c)], writes=[zb_t[c]])
            else:
                S.op("dve", lambda h, c=c: h.tensor_copy(out=zb[:, c, :ntok], in_=z_ap(c)),
                     reads=[z_t(c)], writes=[zb_t[c]])
            S.op("act", lambda h, c=c: h.activation(out=z2b[:, c, :ntok], in_=z_ap(c), func=AF.Square),
                 reads=[z_t(c)], writes=[z2b_t[c]])

        def ln_post(z_ap, z_t, nch, ones_ap, outs, tmp, ntok=GT, post=None):
            zb, zb_t, z2b, z2b_t, rstd, rstd_t, nmr, nmr_t, msq, msq_t, tt_, tt_t = tmp
            pm, pm_t = bank()
            for c in range(nch):
                mm(pm[:, :ntok], pm_t, ones_ap, zb[:, c, :ntok], [zb_t[c], cst_t], c == 0, c == nch - 1)
            pe2, pe2_t = bank()
            for c in range(nch):
                mm(pe2[:, :ntok], pe2_t, ones_ap, z2b[:, c, :ntok], [z2b_t[c], cst_t], c == 0, c == nch - 1)
            S.op("act", lambda h: h.activation(out=msq[:, :ntok], in_=pm[:, :ntok], func=AF.Square),
                 reads=[pm_t], writes=[msq_t])
            S.op("dve", lambda h: h.scalar_tensor_tensor(out=rstd[:, :ntok], in0=pe2[:, :ntok], scalar=EPS,
                                                         in1=msq[:, :ntok], op0=ALU.add, op1=ALU.subtract),
                 reads=[pe2_t, msq_t], writes=[rstd_t])
            S.op("act", lambda h: h.activation(out=rstd[:, :ntok], in_=rstd[:, :ntok], func=AF.Ln),
                 reads=[rstd_t], writes=[rstd_t])
            S.op("act", lambda h: h.activation(out=rstd[:, :ntok], in_=rstd[:, :ntok], func=AF.Exp, scale=-0.5),
                 reads=[rstd_t], writes=[rstd_t])
            S.op("dve", lambda h: h.scalar_tensor_tensor(out=nmr[:, :ntok], in0=pm[:, :ntok], scalar=-1.0,
                                                         in1=rstd[:, :ntok], op0=ALU.mult, op1=ALU.mult),
                 reads=[pm_t, rstd_t], writes=[nmr_t])
            for c in range(nch):
                k = c % len(tt_t)
                S.op("dve", lambda h, c=c, k=k: h.tensor_tensor(out=tt_[:, k, :ntok], in0=z_ap(c),
                                                                in1=rstd[:, :ntok], op=ALU.mult),
                     reads=[z_t(c), rstd_t], writes=[tt_t[k]])
                S.op("pool", lambda h, k=k: h.tensor_tensor(out=tt_[:, k, :ntok], in0=tt_[:, k, :ntok],
                                                            in1=nmr[:, :ntok], op=ALU.add),
                     reads=[tt_t[k], nmr_t], writes=[tt_t[k]])
                for (func, sc, bi, xr, dap, dtl) in outs:
                    S.op("act", lambda h, c=c, k=k, func=func, sc=sc, bi=bi, dap=dap:
                         h.activation(out=dap(c), in_=tt_[:, k, :ntok], func=func, scale=sc(c), bias=bi(c)),
                         reads=[tt_t[k], par_t] + list(xr), writes=[dtl(c)])
                if post is not None and c >= 2:
                    post(c - 2)
            if post is not None:
                for c in range(max(0, nch - 2), nch):
                    post(c)

        def ln_tmps(st, pfx, nch):
            zb = sb(pfx + "zb", [128, nch, GT], BF16, st)
            z2b = sb(pfx + "z2b", [128, nch, GT], BF16, st)
            rstd = sb(pfx + "rstd", [128, GT], F32, st)
            nmr = sb(pfx + "nmr", [128, GT], F32, st)
            nmr_t = T()
            msq, msq_t = nmr, nmr_t
            tt_ = sb(pfx + "tt", [128, 2, GT], F32, st)
            return (zb, [T() for _ in range(nch)], z2b, [T() for _ in range(nch)], rstd, T(), nmr, nmr_t,
                    msq, msq_t, tt_, [T() for _ in range(2)])

        with ExitStack() as st:
            win = sb("win", [128, 8, 1792], BF16, st)
            win_t = T()
            msk = sb("msk", [128, 3 * 512], BF16, st)
            msk_t = T()
            S.dma("pool", msk[:], masks, writes=[msk_t], sem_tile=msk_t)
            mask_own = msk[:, 0:512]
            mask_prev = msk[:, 512:1024]
            mask_first = msk[:, 1024:1536]
            S.dma("pool", win[:], w_in.rearrange("(c p) m -> p c m", p=128), writes=[win_t], sem_tile=win_t)
            diag = sb("diag", [128, 4 * 31, 128], BF16, st)
            diag_t = T()
            S.op("dve", lambda h: h.tensor_tensor(
                out=diag[:, :, :], in0=idf[:, :].unsqueeze(1).to_broadcast([128, 124, 128]),
                in1=par_wdw[:, :].unsqueeze(2).to_broadcast([128, 124, 128]), op=ALU.mult),
                reads=[par_t], writes=[diag_t])
            xb = [sb(f"xb{i}", [128, 8, GT], BF16, st) for i in range(2)]
            xb_t = [T() for _ in range(2)]
            hbuf = sb("hbuf", [128, 4, 32 + GT], BF16, st)
            hh_t = [T() for _ in range(4)]
            hb_t = [T() for _ in range(4)]
            qb = sb("qb", [128, 4, GT], BF16, st)
            qb_t = T()
            kA = sb("kA", [128, 128 + GT], BF16, st)
            kB = sb("kB", [128, 128 + GT], BF16, st)
            kh_t, kb_t = T(), T()
            vz = sb("vz", [128, 5, 2, 128], BF16, st)
            vh_t, vb_t = T(), T()
            sg = sb("sg", [128, 2, GT], F32, st)
            sg_t = [T(), T()]
            yc = sb("yc", [128, 4, GT], F32, st)
            yc_t = [T() for _ in range(4)]
            lt = ln_tmps(st, "c", 4)
            pT = sb("pT", [128, 2, 4, GT], BF16, st)
            pT_t = [[T() for _ in range(4)] for _ in range(2)]
            den = sb("den", [128, 2, 2, GT], F32, st)
            den_t = [[T(), T()], [T(), T()]]
            S.op("pool", lambda h: h.memset(kA[:], 0.0), writes=[kh_t, kb_t])
            S.op("pool", lambda h: h.memset(kB[:], 0.0), writes=[kh_t, kb_t])
            S.op("pool", lambda h: h.memset(vz[:], 0.0), writes=[vh_t, vb_t])
            S.op("pool", lambda h: h.memset(hbuf[:], 0.0), writes=hh_t + hb_t)

            def load_x(gi, buf):
                if gi < 0:
                    src = xT[:, 0:HALO]
                    dst = xb[buf][:, :, 0:HALO]
                else:
                    src = xT[:, HALO + gi * GT: HALO + (gi + 1) * GT]
                    dst = xb[buf][:, :, :]
                S.dma("pool", dst, src.rearrange("(c p) t -> p c t", p=128), writes=[xb_t[buf]],
                      sem_tile=xb_t[buf])

            def inproj(col0, buf, nt, pb, pb_t):
                for k in range(8):
                    mm(pb[:, :nt], pb_t, win[:, k, col0:col0 + 128], xb[buf][:, k, :nt],
                       [win_t, xb_t[buf]], k == 0, k == 7)

            load_x(-1, 0)
            for gi in range(-1, NG):
                buf = (gi + 1) % 2
                nt = HALO if gi < 0 else GT
                if gi + 1 < NG:
                    load_x(gi + 1, (gi + 2) % 2)
                hoff = 32 + (GT - nt)
                koff = 128 + (GT - nt)
                if gi >= 0:
                    for j in range(4):
                        if gi == 0:
                            S.op("pool", lambda h, j=j: h.tensor_scalar(hbuf[:, j, 0:32], hbuf[:, j, GT:GT + 32],
                                                                        par_flag[:, 0:1], None, op0=ALU.mult),
                                 reads=[hb_t[j], par_t], writes=[hh_t[j]])
                        else:
                            S.op("pool", lambda h, j=j: h.tensor_copy(out=hbuf[:, j, 0:32],
                                                                      in_=hbuf[:, j, GT:GT + 32]),
                                 reads=[hb_t[j]], writes=[hh_t[j]])
                    S.op("pool", lambda h: h.tensor_copy(out=kA[:, 0:128], in_=kA[:, GT:GT + 128]),
                         reads=[kb_t], writes=[kh_t])
                    S.op("pool", lambda h: h.tensor_copy(out=kB[:, 0:128], in_=kB[:, GT:GT + 128]),
                         reads=[kb_t], writes=[kh_t])
                    S.op("pool", lambda h: h.tensor_copy(out=vz[:, 0, :, :], in_=vz[:, 4, :, :]),
                         reads=[vb_t], writes=[vh_t])
                for j in range(4):
                    pa, pa_t = bank()
                    inproj(j * 128, buf, nt, pa, pa_t)
                    pg, pg_t = bank()
                    inproj((4 + j) * 128, buf, nt, pg, pg_t)
                    s = j % 2
                    S.op("act", lambda h, j=j, s=s, pg=pg, nt=nt: h.activation(
                        out=sg[:, s, :nt], in_=pg[:, :nt], func=AF.Sigmoid, bias=par_b_in[:, 4 + j:5 + j]),
                        reads=[pg_t, par_t], writes=[sg_t[s]])
                    S.op("dve", lambda h, j=j, s=s, pa=pa, nt=nt, hoff=hoff: h.scalar_tensor_tensor(
                        out=hbuf[:, j, hoff:hoff + nt], in0=pa[:, :nt], scalar=par_b_in[:, j:j + 1],
                        in1=sg[:, s, :nt], op0=ALU.add, op1=ALU.mult),
                        reads=[pa_t, sg_t[s], par_t], writes=[hb_t[j]])
                if gi >= 0:
                    for j in range(4):
                        pq, pq_t = bank()
                        inproj((8 + j) * 128, buf, nt, pq, pq_t)
                        S.op("act", lambda h, j=j, pq=pq: h.activation(
                            out=qb[:, j, :], in_=pq[:, :], func=AF.Identity, bias=par_b_in[:, 8 + j:9 + j]),
                            reads=[pq_t, par_t], writes=[qb_t])
                pk, pk_t = bank()
                inproj(12 * 128, buf, nt, pk, pk_t)
                S.op("act", lambda h, pk=pk, nt=nt, koff=koff: h.activation(
                    out=kA[0:64, koff:koff + nt], in_=pk[0:64, :nt], func=AF.Identity, bias=par_b_in[0:64, 12:13]),
                    reads=[pk_t, par_t], writes=[kb_t])
                S.op("act", lambda h, pk=pk, nt=nt, koff=koff: h.activation(
                    out=kB[64:128, koff:koff + nt], in_=pk[64:128, :nt], func=AF.Identity,
                    bias=par_b_in[64:128, 12:13]),
                    reads=[pk_t, par_t], writes=[kb_t])
                nblk = nt // 128
                for b in range(nblk):
                    slot = 1 + b + (4 - nblk)
                    pv, pv_t = bank()
                    for k in range(8):
                        mm(pv[:, 0:128], pv_t, xb[buf][:, k, b * 128:(b + 1) * 128], win[:, k, 13 * 128:14 * 128],
                           [win_t, xb_t[buf]], k == 0, k == 7)
                    S.op("dve", lambda h, pv=pv, slot=slot: h.tensor_tensor(
                        out=vz[:, slot, 0, 0:64], in0=pv[:, 0:64], in1=par_bv[:, 0:64], op=ALU.add),
                        reads=[pv_t, par_t], writes=[vb_t])
                    S.op("dve", lambda h, pv=pv, slot=slot: h.tensor_tensor(
                        out=vz[:, slot, 1, 64:128], in0=pv[:, 64:128], in1=par_bv[:, 64:128], op=ALU.add),
                        reads=[pv_t, par_t], writes=[vb_t])
                if gi < 0:
                    continue
                g = gi
                gc = slice(g * GT, (g + 1) * GT)
                def conv_chunk(j):
                    pc, pc_t = bank()
                    for tap in range(31):
                        mm(pc[:, :], pc_t, diag[:, j * 31 + tap, :], hbuf[:, j, 2 + tap:2 + tap + GT],
                           [diag_t, hh_t[j], hb_t[j]], tap == 0, tap == 30)
                    S.op("act", lambda h, j=j, pc=pc: h.activation(
                        out=yc[:, j, :], in_=pc[:, :], func=AF.Identity, bias=par_cpar[:, j:j + 1]),
                        reads=[pc_t, par_t], writes=[yc_t[j]])
                    ln_pre(lambda c: yc[:, c, :], lambda c: yc_t[c], j, lt)

                def conv_ln():
                    ln_post(lambda c: yc[:, c, :], lambda c: yc_t[c], 4, ones512,
                            [(AF.Silu, lambda c: par_cpar[:, 4 + c:5 + c], lambda c: par_cpar[:, 8 + c:9 + c], [],
                              lambda c, gc=gc: ymix[:, c, gc], lambda c, g=g: ymT[c][g])], lt)
                def scores(b):
                    bp = b % 2
                    first = (g == 0 and b == 0)
                    qv = qb[:, :, b * 128:(b + 1) * 128]
                    for kv in range(2):
                        kX = kA if kv == 0 else kB
                        for po in range(2):
                            ps_, ps_t = bank()
                            kcol = b * 128 + po * 128
                            kt = kh_t if kcol < 128 else kb_t
                            mm(ps_[:, :], ps_t, kX[:, kcol:kcol + 128], qv, [kt, qb_t], True, False)
                            mk = mask_own if po == 1 else (mask_first if first else mask_prev)
                            mm(ps_[:, :], ps_t, ident_b, mk, [cst_t, msk_t], False, True)
                            idx = kv * 2 + po
                            S.op("act", lambda h, ps_=ps_, idx=idx, bp=bp: h.activation(
                                out=pT[:, bp, idx, :], in_=ps_[:, :], func=AF.Exp, scale=0.125),
                                reads=[ps_t], writes=[pT_t[bp][idx]])

                def pv(b):
                    bp = b % 2
                    pTb = pT[:, bp]
                    po_, po_t = bank()
                    pl_, pl_t = bank()
                    n = 0
                    for kv in range(2):
                        for po in range(2):
                            idx = kv * 2 + po
                            slot = b + po
                            vt = vh_t if slot == 0 else vb_t
                            mm(po_[:, :], po_t, vz[:, slot, kv, :], pTb[:, idx, :], [vt, pT_t[bp][idx]], n == 0, n == 3)
                            n += 1
                    n = 0
                    for kv in range(2):
                        for po in range(2):
                            idx = kv * 2 + po
                            mm(pl_[:, :], pl_t, ones0 if kv == 0 else ones1, pTb[:, idx, :], [cst_t, pT_t[bp][idx]],
                               n == 0, n == 3)
                            n += 1
                    S.op("dve", lambda h, pl_=pl_, bp=bp: h.tensor_tensor(
                        out=den[:, bp, 0, :].rearrange("p (g q) -> p g q", g=4),
                        in0=pl_[:, :].rearrange("p (g q) -> p g q", g=4),
                        in1=sinkexp[:, :].unsqueeze(2).to_broadcast([128, 4, 128]), op=ALU.add),
                        reads=[pl_t, sinkexp_t], writes=[den_t[bp][0]])
                    S.op("act", lambda h, bp=bp: h.activation(out=den[:, bp, 0, :], in_=den[:, bp, 0, :], func=AF.Ln),
                         reads=[den_t[bp][0]], writes=[den_t[bp][0]])
                    S.op("act", lambda h, bp=bp: h.activation(out=den[:, bp, 1, :], in_=den[:, bp, 0, :], func=AF.Exp,
                                                              scale=-1.0),
                         reads=[den_t[bp][0]], writes=[den_t[bp][1]])
                    tc0 = g * GT + b * 128
                    S.op("dve", lambda h, po_=po_, tc0=tc0, bp=bp: h.tensor_tensor(
                        out=ymix[:, 4:8, tc0:tc0 + 128], in0=po_[:, :].rearrange("p (g q) -> p g q", g=4),
                        in1=den[:, bp, 1, :].rearrange("p (g q) -> p g q", g=4), op=ALU.mult),
                        reads=[po_t, den_t[bp][1]], writes=[ymT[4 + j][g] for j in range(4)])

                scores(0)
                for b in range(4):
                    conv_chunk(b)
                    if b + 1 < 4:
                        scores(b + 1)
                    pv(b)
                conv_ln()
            S.flush()
        if stop_after == "A1":
            return nc

        kmT = sb("kmT", [128, 8, 256], BF16)
        kmT_t = T(name="kmT")
        vm = sb("vm", [128, 2, 1024], BF16)
        vm_t = T(name="vm")
        x1d_t = [T(name=f"x1d{g}") for g in range(NG)]
        with ExitStack() as st:
            wout = sb("wout", [128, 8, D], BF16, st)
            wout_t = [T() for _ in range(4)]
            for q4 in range(4):
                S.dma("pool", wout[:, :, q4 * 256:(q4 + 1) * 256],
                      w_out[:, q4 * 256:(q4 + 1) * 256].rearrange("(c p) m -> p c m", p=128),
                      writes=[wout_t[q4]], sem_tile=wout_t[q4])
            memb = sb("memb", [128, 8, 256], BF16, st)
            memb_t = T()
            wk = sb("wk", [128, 8, 1024], BF16, st)
            wk_t = T()
            wv = sb("wv", [128, 8, 1024], BF16, st)
            wv_t = T()
            S.dma("pool", memb[:], memT.rearrange("(c p) m -> p c m", p=128), writes=[memb_t], sem_tile=memb_t)
            S.dma("pool", wk[:], w_mkv[:, 0:1024].rearrange("(c p) m -> p c m", p=128), writes=[wk_t], sem_tile=wk_t)
            S.dma("pool", wv[:], w_mkv[:, 1024:2048].rearrange("(c p) m -> p c m", p=128), writes=[wv_t],
                  sem_tile=wv_t)

            def ph0_compute():
                for c in range(8):
                    pb, pb_t = bank()
                    for k in range(8):
                        mm(pb[:, 0:256], pb_t, wk[:, k, colsl(c)], memb[:, k, :], [wk_t, memb_t], k == 0, k == 7)
                    S.op("act", lambda h, c=c, pb=pb: h.activation(out=kmT[:, c, :], in_=pb[:, 0:256], func=AF.Copy),
                         reads=[pb_t], writes=[kmT_t])
                for mc in range(2):
                    for nh in range(2):
                        pb, pb_t = bank()
                        for k in range(8):
                            mm(pb[:, :], pb_t, memb[:, k, colsl(mc)], wv[:, k, nh * 512:(nh + 1) * 512],
                               [wv_t, memb_t], k == 0, k == 7)
                        S.op("dve", lambda h, mc=mc, nh=nh, pb=pb: h.tensor_copy(
                            out=vm[:, mc, nh * 512:(nh + 1) * 512], in_=pb[:, :]),
                            reads=[pb_t], writes=[vm_t])


            xs = [sb(f"xs{i}", [128, 8, GT], F32, st) for i in range(3)]
            xs_l = [T() for _ in range(3)]
            xs_s = [T() for _ in range(3)]
            xs_t = [[T() for _ in range(8)] for _ in range(3)]
            lt = ln_tmps(st, "l1", 8)
            zb1 = sb("l1zb1", [128, 8, GT], BF16, st)
            z2b1 = sb("l1z2b1", [128, 8, GT], BF16, st)
            ltp = [lt, (zb1, [T() for _ in range(8)], z2b1, [T() for _ in range(8)]) + tuple(lt[4:])]

            def a2_stage1(g):
                i = g % 3
                gc = slice(g * GT, (g + 1) * GT)
                S.dma("sp", xs[i][:], xT[:, HALO + g * GT:HALO + (g + 1) * GT].rearrange("(c p) t -> p c t", p=128),
                      writes=xs_t[i], sem_tile=xs_l[i])
                for c in range(8):
                    pb, pb_t = bank()
                    for k in range(8):
                        mm(pb[:, :], pb_t, wout[:, k, colsl(c)], ymix[:, k, gc], [wout_t[c // 2], ymT[k][g]], k == 0, k == 7)
                    S.op("dve", lambda h, c=c, pb=pb, i=i: h.scalar_tensor_tensor(
                        out=xs[i][:, c, :], in0=xs[i][:, c, :], scalar=ALPHA, in1=pb[:, :],
                        op0=ALU.mult, op1=ALU.add),
                        reads=[pb_t, xs_t[i][c]], writes=[xs_t[i][c]])
                    ln_pre(lambda c, i=i: xs[i][:, c, :], lambda c, i=i: xs_t[i][c], c, ltp[g % 2], cast_eng="act")

            a2_stage1(0)
            for g in range(NG):
                i = g % 3
                gc = slice(g * GT, (g + 1) * GT)
                if g + 1 < NG:
                    a2_stage1(g + 1)

                def post1(c, i=i, gc=gc, g=g):
                    S.op("dve", lambda h: h.tensor_copy(out=ymix[:, c, gc], in_=xs[i][:, c, :]),
                         reads=[xs_t[i][c]], writes=[ymT[c][g]])

                ln_post(lambda c, i=i: xs[i][:, c, :], lambda c, i=i: xs_t[i][c], 8, ones1024,
                        [(AF.Identity, lambda c: par_lnp[:, c:c + 1], lambda c: par_lnp[:, 8 + c:9 + c], [],
                          lambda c, i=i: xs[i][:, c, :], lambda c, i=i: xs_t[i][c])], ltp[g % 2], post=post1)
                S.dma("pool", x1T[:, gc].rearrange("(c p) t -> p c t", p=128), xs[i][:],
                      reads=xs_t[i], writes=[x1d_t[g]], sem_tile=xs_s[i])
            ph0_compute()
            S.flush()
        if stop_after == "A2":
            return nc

        acc = sb("acc", [128, 8, TOK], F32)
        accT = [[T(name=f"acc{c}_{g}") for g in range(NG)] for c in range(8)]

        with ExitStack() as st:
            wmq = sb("wmq", [128, 8, D], BF16, st)
            wmq_t = [T() for _ in range(4)]
            wmo = sb("wmo", [128, 8, D], BF16, st)
            wmo_t = [T() for _ in range(4)]
            for wdst, wsrc, wts in ((wmq, w_mq, wmq_t), (wmo, w_mo, wmo_t)):
                for q4 in range(4):
                    S.dma("pool", wdst[:, :, q4 * 256:(q4 + 1) * 256],
                          wsrc[:, q4 * 256:(q4 + 1) * 256].rearrange("(c p) m -> p c m", p=128),
                          writes=[wts[q4]], sem_tile=wts[q4])
            xs1 = sb("xsb", [128, 8, GT], F32, st)
            xs1_l = T()
            xs1_t = [T() for _ in range(8)]
            qm = sb("qm", [128, 8, GT], BF16, st)
            qm_t = [T() for _ in range(8)]
            om, om_t = qm, qm_t
            pmb = sb("pmb", [128, 1, 2, GT], BF16, st)
            pm_t = [[T(), T()]]
            rl = sb("rl", [128, 1, GT], F32, st)
            rl_t = [T()]
            lt = ln_tmps(st, "l2", 8)
            zb1 = sb("l2zb1", [128, 8, GT], BF16, st)
            z2b1 = sb("l2z2b1", [128, 8, GT], BF16, st)
            ltp = [lt, (zb1, [T() for _ in range(8)], z2b1, [T() for _ in range(8)]) + tuple(lt[4:])]

            def b_stage1(g):
                i = g % 2
                gc = slice(g * GT, (g + 1) * GT)
                S.dma("sp", xs1[:], x1T[:, gc].rearrange("(c p) t -> p c t", p=128),
                      reads=[x1d_t[g]], writes=xs1_t, sem_tile=xs1_l)
                for c in range(8):
                    pb, pb_t = bank()
                    for k in range(8):
                        mm(pb[:, :], pb_t, wmq[:, k, colsl(c)], ymix[:, k, gc], [wmq_t[c // 2], ymT[k][g]], k == 0, k == 7)
                    S.op("act", lambda h, c=c, pb=pb: h.activation(out=qm[:, c, :], in_=pb[:, :], func=AF.Copy),
                         reads=[pb_t], writes=[qm_t[c]])
                for hd in range(4):
                    par = 0
                    for mc in range(2):
                        ps_, ps_t = bank()
                        for dc in range(2):
                            mm(ps_[:, :], ps_t, kmT[:, 2 * hd + dc, colsl(mc)], qm[:, 2 * hd + dc, :],
                               [kmT_t, qm_t[2 * hd + dc]], dc == 0, dc == 1)
                        S.op("act", lambda h, ps_=ps_, par=par, mc=mc: h.activation(
                            out=pmb[:, par, mc, :], in_=ps_[:, :], func=AF.Exp, scale=1.0 / 16.0),
                            reads=[ps_t], writes=[pm_t[par][mc]])
                    pl_, pl_t = bank()
                    for mc in range(2):
                        mm(pl_[:, :], pl_t, onesP, pmb[:, par, mc, :], [cst_t, pm_t[par][mc]],
                           mc == 0, mc == 1)
                    S.op("act", lambda h, pl_=pl_, par=par: h.activation(out=rl[:, par, :], in_=pl_[:, :], func=AF.Ln),
                         reads=[pl_t], writes=[rl_t[par]])
                    S.op("act", lambda h, par=par: h.activation(out=rl[:, par, :], in_=rl[:, par, :], func=AF.Exp,
                                                                scale=-1.0),
                         reads=[rl_t[par]], writes=[rl_t[par]])
                    for dc in range(2):
                        po_, po_t = bank()
                        for mc in range(2):
                            mm(po_[:, :], po_t, vm[:, mc, colsl(2 * hd + dc)], pmb[:, par, mc, :],
                               [vm_t, pm_t[par][mc]], mc == 0, mc == 1)
                        S.op("dve", lambda h, po_=po_, par=par, hd=hd, dc=dc: h.tensor_tensor(
                            out=om[:, 2 * hd + dc, :], in0=po_[:, :], in1=rl[:, par, :], op=ALU.mult),
                            reads=[po_t, rl_t[par]], writes=[om_t[2 * hd + dc]])
                for c in range(8):
                    pb, pb_t = bank()
                    for k in range(8):
                        mm(pb[:, :], pb_t, wmo[:, k, colsl(c)], om[:, k, :], [wmo_t[c // 2], om_t[k]], k == 0, k == 7)
                    S.op("dve", lambda h, c=c, pb=pb, gc=gc: h.scalar_tensor_tensor(
                        out=acc[:, c, gc], in0=xs1[:, c, :], scalar=ALPHA, in1=pb[:, :],
                        op0=ALU.mult, op1=ALU.add),
                        reads=[pb_t, xs1_t[c]], writes=[accT[c][g]])
                    ln_pre(lambda c, gc=gc: acc[:, c, gc], lambda c, g=g: accT[c][g], c, ltp[i], cast_eng="act")

            b_stage1(0)
            for g in range(NG):
                i = g % 2
                gc = slice(g * GT, (g + 1) * GT)
                if g + 1 < NG:
                    b_stage1(g + 1)
                def post2(c, gc=gc, g=g):
                    S.op("dve", lambda h: h.tensor_scalar(ymix[:, c, gc], acc[:, c, gc], 1.0 / ALPHA, None, op0=ALU.mult),
                         reads=[accT[c][g]], writes=[ymT[c][g]])

                ln_post(lambda c, gc=gc: acc[:, c, gc], lambda c, g=g: accT[c][g], 8, ones1024,
                        [(AF.Identity, lambda c: par_lnpa[:, c:c + 1], lambda c: par_lnpa[:, 8 + c:9 + c], [lnpa_t],
                          lambda c, gc=gc: acc[:, c, gc], lambda c, g=g: accT[c][g])], ltp[i], post=post2)
            S.flush()
        if stop_after == "B":
            S.dma("sp", outT.rearrange("(c p) t -> p c t", p=128), acc[:],
                  reads=[x for row in accT for x in row], writes=[], sem_tile=T())
            S.flush()
            return nc

        with ExitStack() as st:
            wrt = sb("wrt", [128, 8, 20], F32, st)
            wrt_t = T()
            S.dma("sp", wrt[:], w_rt.rearrange("p (c n) -> p c n", c=8), writes=[wrt_t], sem_tile=wrt_t)
            selt = sb("selt", [16, 16, 128], F32, st)
            selt_t = T()
            S.dma("sp", selt[:], sel.rearrange("k (e m) -> k e m", e=16), writes=[selt_t], sem_tile=selt_t)
            wg = [sb(f"wg{i}", [128, 8, 512], BF16, st) for i in range(2)]
            wu = [sb(f"wu{i}", [128, 8, 512], BF16, st) for i in range(2)]
            wd = [sb(f"wd{i}", [128, 4, D], BF16, st) for i in range(2)]
            wg_t = [T() for _ in range(2)]
            wu_t = [T() for _ in range(2)]
            wd_t = [T() for _ in range(2)]
            cbc = sb("cbc", [128, 2, GT], F32, st)
            cbc_t = [T(), T()]
            sgm = sb("sgm", [128, 2, GT], F32, st)
            sgm_t = [T(), T()]
            tm = sb("tm", [128, 2, GT], F32, st)
            tm_t = [T(), T()]
            hh = sb("hh", [128, 2, 4, GT], BF16, st)
            hh_t2 = [[T() for _ in range(4)] for _ in range(2)]

            def load_expert(e):
                i = e % 2
                S.dma("pool", wg[i][:], w_gate[e].rearrange("(c p) f -> p c f", p=128), writes=[wg_t[i]],
                      sem_tile=wg_t[i])
                S.dma("pool", wu[i][:], w_up[e].rearrange("(c p) f -> p c f", p=128), writes=[wu_t[i]],
                      sem_tile=wu_t[i])
                S.dma("pool", wd[i][:], w_down[e].rearrange("(c p) f -> p c f", p=128), writes=[wd_t[i]],
                      sem_tile=wd_t[i])

            load_expert(0)
            lg = sb("lg", [128, 16, 20], F32, st)
            lg_t = T()
            lgT = wg[1][:].bitcast(F32).rearrange("p c f -> p (c f)")
            for g in range(NG):
                gc = slice(g * GT, (g + 1) * GT)
                pb, pb_t = bank()
                for k in range(8):
                    mm(pb[0:20, :], pb_t, wrt[:, k, :], acc[:, k, gc], [accT[k][g], wrt_t], k == 0, k == 7)
                S.op("act", lambda h, pb=pb, gc=gc: h.activation(
                    out=lgT[0:20, gc], in_=pb[0:20, :], func=AF.Identity, scale=1.0 / ALPHA, bias=par_brc[0:20, 0:1]),
                    reads=[pb_t, par_t], writes=[wg_t[1]])
            for g in range(NG):
                pb, pb_t = bank()
                for q in range(4):
                    tt = g * 4 + q
                    S.op("pe", lambda h, pb=pb, q=q, tt=tt: h.transpose(
                        out=pb[:, q * 20:(q + 1) * 20], in_=lgT[0:20, tt * 128:(tt + 1) * 128],
                        identity=idf[0:20, 0:20]),
                        reads=[wg_t[1], par_t], writes=[pb_t], inc=True)
                S.op("dve", lambda h, pb=pb, g=g: h.tensor_copy(
                    out=lg[:, 4 * g:4 * g + 4, :].rearrange("p a b -> p (a b)"), in_=pb[:, 0:80]),
                    reads=[pb_t], writes=[lg_t])
            R = sb("R", [128, 16, 64], F32, st)
            R_t = T()

            def rop(eng, fn):
                S.op(eng, fn, reads=[lg_t, R_t], writes=[R_t])

            def b3(ap2, n):
                return ap2.unsqueeze(2).to_broadcast([128, 16, n])

            gl = lg[:, :, 0:4]
            gmax, gsum, gp = R[:, :, 0], R[:, :, 1], R[:, :, 2]
            gm = R[:, :, 4:8]
            gd = R[:, :, 8:12]
            el = R[:, :, 12:16]
            tmp = R[:, :, 16:20]
            m1, m2, d21, s1, s2 = R[:, :, 20], R[:, :, 21], R[:, :, 22], R[:, :, 23], R[:, :, 24]
            mk1 = R[:, :, 28:32]
            el2 = R[:, :, 32:36]
            mk2 = R[:, :, 36:40]
            cg = R[:, :, 40:44]
            comb = R[:, :, 48:64]
            rop("dve", lambda h: h.tensor_reduce(out=gmax, in_=gl, axis=AX.X, op=ALU.max))
            rop("dve", lambda h: h.tensor_tensor(out=gm, in0=gl, in1=b3(gmax, 4), op=ALU.is_equal))
            rop("dve", lambda h: h.tensor_tensor(out=gd, in0=gl, in1=b3(gmax, 4), op=ALU.subtract))
            rop("act", lambda h: h.activation(out=gd, in_=gd, func=AF.Exp))
            rop("dve", lambda h: h.tensor_reduce(out=gsum, in_=gd, axis=AX.X, op=ALU.add))
            rop("dve", lambda h: h.reciprocal(out=gp, in_=gsum))
            for gg in range(4):
                dst = el if gg == 0 else tmp
                rop("dve", lambda h, gg=gg, dst=dst: h.tensor_tensor(
                    out=dst, in0=lg[:, :, 4 + 4 * gg:8 + 4 * gg], in1=b3(R[:, :, 4 + gg], 4), op=ALU.mult))
                if gg > 0:
                    rop("dve", lambda h: h.tensor_tensor(out=el, in0=el, in1=tmp, op=ALU.add))
            rop("dve", lambda h: h.tensor_reduce(out=m1, in_=el, axis=AX.X, op=ALU.max))
            rop("dve", lambda h: h.tensor_tensor(out=mk1, in0=el, in1=b3(m1, 4), op=ALU.is_equal))
            rop("dve", lambda h: h.scalar_tensor_tensor(out=el2, in0=mk1, scalar=-1.0e30, in1=el,
                                                        op0=ALU.mult, op1=ALU.add))
            rop("dve", lambda h: h.tensor_reduce(out=m2, in_=el2, axis=AX.X, op=ALU.max))
            rop("dve", lambda h: h.tensor_tensor(out=mk2, in0=el2, in1=b3(m2, 4), op=ALU.is_equal))
            rop("dve", lambda h: h.tensor_tensor(out=d21, in0=m2, in1=m1, op=ALU.subtract))
            rop("act", lambda h: h.activation(out=d21, in_=d21, func=AF.Exp))
            rop("dve", lambda h: h.tensor_scalar(s1, d21, 1.0, None, op0=ALU.add))
            rop("dve", lambda h: h.reciprocal(out=s1, in_=s1))
            rop("dve", lambda h: h.tensor_tensor(out=s2, in0=d21, in1=s1, op=ALU.mult))
            rop("dve", lambda h: h.tensor_tensor(out=s1, in0=s1, in1=gp, op=ALU.mult))
            rop("dve", lambda h: h.tensor_tensor(out=s2, in0=s2, in1=gp, op=ALU.mult))
            rop("dve", lambda h: h.tensor_tensor(out=cg, in0=mk1, in1=b3(s1, 4), op=ALU.mult))
            rop("dve", lambda h: h.tensor_tensor(out=tmp, in0=mk2, in1=b3(s2, 4), op=ALU.mult))
            rop("dve", lambda h: h.tensor_tensor(out=cg, in0=cg, in1=tmp, op=ALU.add))
            for gg in range(4):
                rop("dve", lambda h, gg=gg: h.tensor_tensor(
                    out=R[:, :, 48 + 4 * gg:52 + 4 * gg], in0=cg, in1=b3(R[:, :, 4 + gg], 4), op=ALU.mult))
            combT = sb("combT", [16, TOK], F32, st)
            combT_t = [T() for _ in range(NG)]
            for g in range(NG):
                pb, pb_t = bank()
                for q in range(4):
                    tt = g * 4 + q
                    S.op("pe", lambda h, pb=pb, q=q, tt=tt: h.transpose(
                        out=pb[0:16, q * 128:(q + 1) * 128], in_=R[:, tt, 48:64], identity=idf[:]),
                        reads=[R_t, par_t], writes=[pb_t], inc=True)
                S.op("act", lambda h, pb=pb, g=g: h.activation(
                    out=combT[:, g * GT:(g + 1) * GT], in_=pb[0:16, :], func=AF.Copy),
                    reads=[pb_t], writes=[combT_t[g]])
            ob = sb("ob", [128, 2, 2, GT], F32, st)
            ob_l = [T(), T()]
            ob_t = [T(), T()]
            wdv = wd[0][:].bitcast(F32)
            l3n_t = T()
            lt3 = (wg[0], [T() for _ in range(8)], wu[0], [T() for _ in range(8)], wdv[:, 0, :], T(),
                   wdv[:, 1, :], l3n_t, wdv[:, 1, :], l3n_t, wdv[:, 2:4, :], [T(), T()])
            ln3_state = {"fenced": False}

            def ln3(g):
                gc = slice(g * GT, (g + 1) * GT)
                if not ln3_state["fenced"]:
                    ln3_state["fenced"] = True
                    allt = [wg_t[0], wu_t[0], wd_t[0]] + lt3[1] + lt3[3] + [lt3[5], lt3[7]] + lt3[11]
                    S.op("pool", lambda h: h.memset(wdv[:, 0, 0:2], 0.0), writes=allt)

                def post(c):
                    if c % 2 == 1:
                        bf = (c // 2) % 2
                        S.dma("sp", outT[(c - 1) * 128:(c + 1) * 128, gc].rearrange("(c p) t -> p c t", p=128),
                              ob[:, bf, :, :], reads=[ob_t[bf]], writes=[], sem_tile=ob_l[bf])

                layernorm(lambda c, gc=gc: acc[:, c, gc], lambda c, g=g: accT[c][g], 8, ones1024,
                          [(AF.Identity, lambda c: par_lnp[:, 32 + c:33 + c], lambda c: par_lnp[:, 40 + c:41 + c], [],
                            lambda c: ob[:, (c // 2) % 2, c % 2, :], lambda c: ob_t[(c // 2) % 2])], lt3, post=post)

            def gateup(e, g, ci):
                i = e % 2
                gc = slice(g * GT, (g + 1) * GT)
                pcb, pcb_t = bank()
                mm(pcb[:, :], pcb_t, selt[:, e, :], combT[:, gc], [selt_t, combT_t[g]], True, True)
                S.op("act", lambda h, pcb=pcb, ci=ci: h.activation(out=cbc[:, ci, :], in_=pcb[:, :], func=AF.Copy),
                     reads=[pcb_t], writes=[cbc_t[ci]])
                for fc in range(4):
                    s_ = fc % 2
                    pg_, pg_t = bank()
                    for k in range(8):
                        mm(pg_[:, :], pg_t, wg[i][:, k, colsl(fc)], ymix[:, k, gc], [wg_t[i], ymT[k][g]],
                           k == 0, k == 7)
                    pu_, pu_t = bank()
                    for k in range(8):
                        mm(pu_[:, :], pu_t, wu[i][:, k, colsl(fc)], ymix[:, k, gc], [wu_t[i], ymT[k][g]],
                           k == 0, k == 7)
                    S.op("act", lambda h, pg_=pg_, s_=s_: h.activation(out=sgm[:, s_, :], in_=pg_[:, :], func=AF.Silu),
                         reads=[pg_t], writes=[sgm_t[s_]])
                    S.op("dve", lambda h, pu_=pu_, s_=s_: h.tensor_tensor(
                        out=tm[:, s_, :], in0=sgm[:, s_, :], in1=pu_[:, :], op=ALU.mult),
                        reads=[pu_t, sgm_t[s_]], writes=[tm_t[s_]])
                    S.op("pool", lambda h, s_=s_, ci=ci, fc=fc: h.tensor_tensor(
                        out=hh[:, ci, fc, :], in0=tm[:, s_, :], in1=cbc[:, ci, :], op=ALU.mult),
                        reads=[tm_t[s_], cbc_t[ci]], writes=[hh_t2[ci][fc]])

            def down(e, g, ci):
                i = e % 2
                gc = slice(g * GT, (g + 1) * GT)
                for dc in range(8):
                    pd_, pd_t = bank()
                    for fc in range(4):
                        mm(pd_[:, :], pd_t, wd[i][:, fc, colsl(dc)], hh[:, ci, fc, :], [wd_t[i], hh_t2[ci][fc]],
                           fc == 0, fc == 3)
                    S.op("dve", lambda h, pd_=pd_, dc=dc, gc=gc: h.tensor_tensor(
                        out=acc[:, dc, gc], in0=acc[:, dc, gc], in1=pd_[:, :], op=ALU.add),
                        reads=[pd_t, accT[dc][g]], writes=[accT[dc][g]])
                if e == 15 and g >= 1:
                    ln3(g - 1)

            units = [(e, g) for e in range(16) for g in range(NG)]
            for u, (e, g) in enumerate(units):
                gateup(e, g, u % 2)
                if u >= 1:
                    pe_, pg2 = units[u - 1]
                    down(pe_, pg2, (u - 1) % 2)
                if g == 0 and e + 1 < 16:
                    load_expert(e + 1)
            down(15, NG - 1, (len(units) - 1) % 2)
            ln3(NG - 1)
            S.flush()
    return nc


def _chunked(v, n):
    return np.ascontiguousarray(np.asarray(v, np.float32).reshape(n, 128).T)


def prepare_inputs(inp):
    f = lambda k: np.asarray(inp[k], np.float32)
    x, mem = f("x"), f("mem")
    w_in, b_in = f("w_in")[0], f("b_in")[0]
    qperm = np.empty(512, np.int64)
    for j in range(4):
        for r in range(2):
            qperm[j * 128 + r * 64:(j * 128 + r * 64 + 64)] = (r * 4 + j) * 64 + np.arange(64)
    cols = np.concatenate([np.arange(1024), 1024 + qperm, np.arange(1536, 1792)])
    w_in_p = np.ascontiguousarray(w_in[:, cols])
    b_in_p = b_in[cols]
    shared = {
        "w_in": w_in_p,
        "b_in": _chunked(b_in_p, 14),
        "bv_rep": np.ascontiguousarray(np.tile(b_in_p[1664:1792][None, :], (128, 1))),
        "wdw": np.ascontiguousarray(f("w_dw")[0].reshape(31, 4, 128).transpose(2, 1, 0).reshape(128, 124)),
        "cpar": np.concatenate([_chunked(f("b_dw")[0], 4), _chunked(f("g_conv_norm")[0], 4),
                                _chunked(f("b_conv_norm")[0], 4)], axis=1),
        "w_out": np.ascontiguousarray(f("w_out")[0][np.concatenate([np.arange(512), 512 + qperm])]),
        "lnp": np.concatenate([_chunked(f(k)[0], 8) for k in ("g_ln1", "b_ln1", "g_ln2", "b_ln2", "g_ln3", "b_ln3")],
                              axis=1),
        "w_mq": f("w_mq")[0], "w_mkv": f("w_mkv")[0], "w_mo": f("w_mo")[0],
        "w_gate": f("w_gate")[0], "w_up": f("w_up")[0], "w_down": f("w_down")[0],
    }
    sk = f("attn_sinks")[0]
    shared["sinks"] = np.ascontiguousarray(np.concatenate([np.tile(sk[None, 0:4], (64, 1)),
                                                           np.tile(sk[None, 4:8], (64, 1))], axis=0))
    wr = np.concatenate([f("w_group")[0]] + [f("w_router")[0][g] for g in range(4)], axis=1)
    shared["w_rt"] = np.ascontiguousarray(wr.reshape(8, 128, 20).transpose(1, 0, 2).reshape(128, 160))
    br = np.concatenate([f("b_group")[0], f("b_router")[0].reshape(-1)])
    shared["b_rt"] = np.ascontiguousarray(np.tile(br[None, :], (128, 1)))
    shared["b_rt_col"] = np.ascontiguousarray(br.reshape(20, 1))
    kk = np.arange(128)[:, None]
    qq = np.arange(128)[None, :]
    m_own = np.where(kk <= qq, 0.0, NEG).astype(np.float32)
    m_prev = np.where(kk > qq, 0.0, NEG).astype(np.float32)
    m_none = np.full((128, 128), NEG, np.float32)
    eye = np.eye(128, dtype=np.float32)
    o0 = np.zeros((128, 128), np.float32); o0[:, :64] = 1.0
    o1 = np.zeros((128, 128), np.float32); o1[:, 64:] = 1.0
    on = np.ones((128, 128), np.float32)
    shared["consts"] = np.ascontiguousarray(np.concatenate([eye, o0, o1, on / 512.0, on / 1024.0, on], axis=1))
    shared["identf"] = eye
    sel = np.zeros((16, 16, 128), np.float32)
    for e in range(16):
        sel[e, e, :] = 1.0
    shared["sel"] = sel.reshape(16, 2048)
    in_maps = []
    for c in range(NCORES):
        b, s0 = c // 4, (c % 4) * TOK
        halo = x[b, s0 - HALO:s0] if s0 > 0 else np.zeros((HALO, D), np.float32)
        m = dict(shared)
        m["xT"] = np.ascontiguousarray(np.concatenate([halo, x[b, s0:s0 + TOK]], axis=0).T)
        m["flag"] = np.full((128, 1), 1.0 if s0 > 0 else 0.0, np.float32)
        m["memT"] = np.ascontiguousarray(mem[b].T)
        mf = m_prev if s0 > 0 else m_none
        m["masks"] = np.ascontiguousarray(np.concatenate([np.tile(m_own, (1, 4)), np.tile(m_prev, (1, 4)),
                                                          np.tile(mf, (1, 4))], axis=1))
        in_maps.append(m)
    return in_maps


_NC_CACHE = {}


def kernel(**inputs):
    in_maps = prepare_inputs(inputs)
    if "nc" not in _NC_CACHE:
        _NC_CACHE["nc"] = build_program()
    nc = _NC_CACHE["nc"]
    res = run_bass_kernel_spmd(nc, in_maps, core_ids=list(range(NCORES)))
    out = np.empty((2, SEQ, D), np.float32)
    for c in range(NCORES):
        b, s0 = c // 4, (c % 4) * TOK
        out[b, s0:s0 + TOK, :] = np.asarray(res.results[c]["outT"], np.float32).T
    return out
```

```python
import numpy as np
from contextlib import ExitStack
import concourse.bass as bass
import concourse.mybir as mybir
from concourse.bass_utils import run_bass_kernel_spmd

F32, BF16 = mybir.dt.float32, mybir.dt.bfloat16
AF = mybir.ActivationFunctionType
ALU = mybir.AluOpType
AX = mybir.AxisListType

NCORES = 8
D = 1024
SEQ = 8192
TOK = 2048
NG = 4
GT = 512
HALO = 128
XW = HALO + TOK
ALPHA = 2.0 ** 0.25
EPS = 1e-5
NEG = -30000.0
NSEM = 48
DEBUG = False


class T:
    __slots__ = ("ap", "w", "r", "dsem", "dcnt", "name", "wsame")

    def __init__(self, ap=None, name=""):
        self.ap = ap
        self.w = None
        self.r = {}
        self.dsem = None
        self.dcnt = 0
        self.name = name


class Eng:
    def __init__(self, name, sem):
        self.name = name
        self.sem = sem
        self.count = 0
        self.observed = {}
        self.ops = []


class Sched:
    def __init__(self, nc, stack):
        self.nc = nc
        self.stack = stack
        self.semn = 0
        self.sem_pool = [self.stack.enter_context(self.nc.semaphore(f"s{i}")) for i in range(NSEM)]
        self.engs = {}
        for n in ("pe", "act", "dve", "pool", "sp"):
            self.engs[n] = Eng(n, self.newsem("e_" + n))
        self.pending_dma = {}
        pool = list(self.sem_pool)
        with self.nc.Block() as block:
            @block.sync
            def _(h):
                for sm in pool:
                    h.sem_clear(sm)

    def newsem(self, name):
        self.semn += 1
        return self.sem_pool[self.semn - 1]

    def _collect(self, e, reads, writes):
        deps = {}

        def add(tok, raw):
            if tok is None:
                return
            sem, val = tok
            if sem is e.sem:
                if e.name == "pe":
                    return
            k = sem.name if hasattr(sem, "name") else id(sem)
            k = id(sem)
            if e.observed.get(k, 0) >= val:
                return
            if k not in deps or deps[k][1] < val:
                deps[k] = (sem, val)

        for t in reads:
            add(t.w, True)
        for t in writes:
            add(t.w, False)
            for tok in t.r.values():
                add(tok, False)
        waits = list(deps.values())
        for sem, val in waits:
            e.observed[id(sem)] = val
        return waits

    def op(self, eng, fn, reads=(), writes=(), inc=True, attach=True):
        e = self.engs[eng]
        waits = self._collect(e, reads, writes)
        if inc:
            e.count += 1
            tok = (e.sem, e.count)
        else:
            tok = (e.sem, e.count + 1)
        for t in reads:
            t.r[id(e.sem)] = tok
        for t in writes:
            t.w = tok
            t.r = {}
        esem = e.sem
        can_attach = attach and eng in ("act", "dve", "pool")

        def emit(h):
            ws = list(waits)
            last = ws.pop() if (ws and can_attach) else None
            for sem, val in ws:
                h.wait_ge(sem, val)
            ins = fn(h)
            if last is not None:
                ins._wait_ge(last[0], last[1])
            if inc:
                ins.then_inc(esem, 1)

        e.ops.append(emit)

    def dma(self, queue, out_ap, in_ap, reads=(), writes=(), sem_tile=None, **kw):
        e = self.engs[queue]
        waits = self._collect(e, reads, writes)
        st = sem_tile
        if st.dsem is None:
            st.dsem = self.newsem("d")
        st.dcnt += 1
        tok = (st.dsem, 16 * st.dcnt)
        for t in reads:
            t.r[id(st.dsem)] = tok
        for t in writes:
            t.w = tok
            t.r = {}
        self.pending_dma[id(st.dsem)] = tok
        dsem = st.dsem

        def emit(h):
            for sem, val in waits:
                h.wait_ge(sem, val)
            h.dma_start(out=out_ap, in_=in_ap, **kw).then_inc(dsem, 16)

        e.ops.append(emit)

    def wait_all_dma(self, eng="sp"):
        e = self.engs[eng]
        toks = list(self.pending_dma.values())
        self.pending_dma = {}

        def emit(h):
            for sem, val in toks:
                h.wait_ge(sem, val)

        e.ops.append(emit)

    def flush(self):
        self.wait_all_dma("sp")
        with self.nc.Block() as block:
            for name, deco in (("pool", block.gpsimd), ("pe", block.tensor), ("act", block.scalar),
                               ("dve", block.vector), ("sp", block.sync)):
                ops = self.engs[name].ops

                def body(h, ops=ops):
                    for f in ops:
                        f(h)

                deco(body)
                self.engs[name].ops = []


def build_program(stop_after=None):
    nc = bass.Bass("TRN2", target_bir_lowering=False)
    dt = nc.dram_tensor

    def din(name, shape, dtype=F32):
        return dt(name, list(shape), dtype, kind="ExternalInput").ap()

    xT = din("xT", [D, XW])
    flag = din("flag", [128, 1])
    memT = din("memT", [D, 256])
    w_in = din("w_in", [D, 1792])
    b_in = din("b_in", [128, 14])
    bv_rep = din("bv_rep", [128, 128])
    wdw = din("wdw", [128, 4 * 31])
    cpar = din("cpar", [128, 12])
    sinks = din("sinks", [128, 4])
    w_out = din("w_out", [D, D])
    lnp = din("lnp", [128, 48])
    w_mq = din("w_mq", [D, D])
    w_mkv = din("w_mkv", [D, 2 * D])
    w_mo = din("w_mo", [D, D])
    w_rt = din("w_rt", [128, 8 * 20])
    b_rt = din("b_rt", [128, 20])
    b_rt_col = din("b_rt_col", [20, 1])
    w_gate = din("w_gate", [16, D, 512])
    w_up = din("w_up", [16, D, 512])
    w_down = din("w_down", [16, 512, D])
    masks = din("masks", [128, 3 * 512])
    consts = din("consts", [128, 6 * 128])
    identf = din("identf", [128, 128])
    sel = din("sel", [16, 16 * 128])
    outT = dt("outT", [D, TOK], F32, kind="ExternalOutput").ap()
    x1T = dt("x1T", [D, TOK], F32, kind="ExternalOutput" if DEBUG else "Internal").ap()

    with ExitStack() as stack:
        S = Sched(nc, stack)
        ec = stack.enter_context

        def sb(name, shape, dtype, st=None):
            return (st or stack).enter_context(nc.sbuf_tensor(name, list(shape), dtype))

        banks = []
        for i in range(8):
            p = ec(nc.psum_tensor(f"ps{i}", [128, 512], F32))
            banks.append((p, T(name=f"ps{i}")))
        bstate = {"i": 0}

        def bank():
            b = banks[bstate["i"] % 8]
            bstate["i"] += 1
            return b

        def mm(out_ap, outT_, lhsT, rhs, reads, start, stop):
            S.op("pe", lambda h: h.matmul(out_ap, lhsT=lhsT, rhs=rhs, start=start, stop=stop),
                 reads=reads, writes=[outT_], inc=stop)

        ymix = sb("ymix", [128, 8, TOK], BF16)
        ymT = [[T(name=f"ym{c}_{g}") for g in range(NG)] for c in range(8)]
        par_b_in = sb("p_b_in", [128, 14], F32)
        par_cpar = sb("p_cpar", [128, 12], F32)
        par_sinks = sb("p_sinks", [128, 4], F32)
        par_lnp = sb("p_lnp", [128, 48], F32)
        par_lnpa = sb("p_lnpa", [128, 16], F32)
        par_flag = sb("p_flag", [128, 1], F32)
        par_bv = sb("p_bv", [128, 128], F32)
        par_wdw = sb("p_wdw", [128, 4 * 31], F32)
        par_brt = sb("p_brt", [128, 20], F32)
        par_brc = sb("p_brc", [20, 1], F32)
        cst = sb("cst", [128, 6 * 128], BF16)
        idf = sb("idf", [128, 128], F32)
        par_t = T(name="params")
        ident_b = cst[:, 0:128]
        ones0 = cst[:, 128:256]
        ones1 = cst[:, 256:384]
        ones512 = cst[:, 384:512]
        ones1024 = cst[:, 512:640]
        onesP = cst[:, 640:768]

        plist = [(par_b_in, b_in, "sp"), (par_cpar, cpar, "sp"), (par_sinks, sinks, "sp"), (par_lnp, lnp, "sp"),
                 (par_flag, flag, "sp"), (par_bv, bv_rep, "sp"), (par_wdw, wdw, "sp"), (par_brt, b_rt, "sp"), (par_brc, b_rt_col, "sp"),
                 (idf, identf, "sp"), (cst, consts, "pool")]
        cst_t = T(name="consts")
        for dst, src, q in plist:
            S.dma(q, dst[:], src, writes=[], sem_tile=(par_t if q == "sp" else cst_t))
        par_t.w = (par_t.dsem, 16 * par_t.dcnt)
        cst_t.w = (cst_t.dsem, 16 * cst_t.dcnt)
        sinkexp = sb("sinkexp", [128, 4], F32)
        sinkexp_t = T(name="sinkexp")
        S.op("act", lambda h: h.activation(out=sinkexp[:], in_=par_sinks[:], func=AF.Exp),
             reads=[par_t], writes=[sinkexp_t])
        lnpa_t = T(name="lnpa")
        S.op("dve", lambda h: h.tensor_scalar(par_lnpa[:], par_lnp[:, 16:32], ALPHA, None, op0=ALU.mult),
             reads=[par_t], writes=[lnpa_t])

        def colsl(c):
            return slice(c * 128, (c + 1) * 128)

        def layernorm(z_ap, z_t, nch, ones_ap, outs, tmp, ntok=GT, post=None):
            for c in range(nch):
                ln_pre(z_ap, z_t, c, tmp, ntok)
            ln_post(z_ap, z_t, nch, ones_ap, outs, tmp, ntok, post)

        def ln_pre(z_ap, z_t, c, tmp, ntok=GT, cast_eng="dve"):
            zb, zb_t, z2b, z2b_t = tmp[0:4]
            if cast_eng == "act":
                S.op("act", lambda h, c=c: h.activation(out=zb[:, c, :ntok], in_=z_ap(c), func=AF.Copy),
                     reads=[z_t(c)], writes=[zb_t[c]])
            else:
                S.op("dve", lambda h, c=c: h.tensor_copy(out=zb[:, c, :ntok], in_=z_ap(c)),
                     reads=[z_t(c)], writes=[zb_t[c]])
            S.op("act", lambda h, c=c: h.activation(out=z2b[:, c, :ntok], in_=z_ap(c), func=AF.Square),
                 reads=[z_t(c)], writes=[z2b_t[c]])

        def ln_post(z_ap, z_t, nch, ones_ap, outs, tmp, ntok=GT, post=None):
            zb, zb_t, z2b, z2b_t, rstd, rstd_t, nmr, nmr_t, msq, msq_t, tt_, tt_t = tmp
            pm, pm_t = bank()
            for c in range(nch):
                mm(pm[:, :ntok], pm_t, ones_ap, zb[:, c, :ntok], [zb_t[c], cst_t], c == 0, c == nch - 1)
            pe2, pe2_t = bank()
            for c in range(nch):
                mm(pe2[:, :ntok], pe2_t, ones_ap, z2b[:, c, :ntok], [z2b_t[c], cst_t], c == 0, c == nch - 1)
            S.op("act", lambda h: h.activation(out=msq[:, :ntok], in_=pm[:, :ntok], func=AF.Square),
                 reads=[pm_t], writes=[msq_t])
            S.op("dve", lambda h: h.scalar_tensor_tensor(out=rstd[:, :ntok], in0=pe2[:, :ntok], scalar=EPS,
                                                         in1=msq[:, :ntok], op0=ALU.add, op1=ALU.subtract),
                 reads=[pe2_t, msq_t], writes=[rstd_t])
            S.op("act", lambda h: h.activation(out=rstd[:, :ntok], in_=rstd[:, :ntok], func=AF.Ln),
                 reads=[rstd_t], writes=[rstd_t])
            S.op("act", lambda h: h.activation(out=rstd[:, :ntok], in_=rstd[:, :ntok], func=AF.Exp, scale=-0.5),
                 reads=[rstd_t], writes=[rstd_t])
            S.op("dve", lambda h: h.scalar_tensor_tensor(out=nmr[:, :ntok], in0=pm[:, :ntok], scalar=-1.0,
                                                         in1=rstd[:, :ntok], op0=ALU.mult, op1=ALU.mult),
                 reads=[pm_t, rstd_t], writes=[nmr_t])
            for c in range(nch):
                k = c % len(tt_t)
                S.op("dve", lambda h, c=c, k=k: h.tensor_tensor(out=tt_[:, k, :ntok], in0=z_ap(c),
                                                                in1=rstd[:, :ntok], op=ALU.mult),
                     reads=[z_t(c), rstd_t], writes=[tt_t[k]])
                S.op("pool", lambda h, k=k: h.tensor_tensor(out=tt_[:, k, :ntok], in0=tt_[:, k, :ntok],
                                                            in1=nmr[:, :ntok], op=ALU.add),
                     reads=[tt_t[k], nmr_t], writes=[tt_t[k]])
                for (func, sc, bi, xr, dap, dtl) in outs:
                    S.op("act", lambda h, c=c, k=k, func=func, sc=sc, bi=bi, dap=dap:
                         h.activation(out=dap(c), in_=tt_[:, k, :ntok], func=func, scale=sc(c), bias=bi(c)),
                         reads=[tt_t[k], par_t] + list(xr), writes=[dtl(c)])
                if post is not None and c >= 2:
                    post(c - 2)
            if post is not None:
                for c in range(max(0, nch - 2), nch):
                    post(c)

        def ln_tmps(st, pfx, nch):
            zb = sb(pfx + "zb", [128, nch, GT], BF16, st)
            z2b = sb(pfx + "z2b", [128, nch, GT], BF16, st)
            rstd = sb(pfx + "rstd", [128, GT], F32, st)
            nmr = sb(pfx + "nmr", [128, GT], F32, st)
            nmr_t = T()
            msq, msq_t = nmr, nmr_t
            tt_ = sb(pfx + "tt", [128, 2, GT], F32, st)
            return (zb, [T() for _ in range(nch)], z2b, [T() for _ in range(nch)], rstd, T(), nmr, nmr_t,
                    msq, msq_t, tt_, [T() for _ in range(2)])

        with ExitStack() as st:
            win = sb("win", [128, 8, 1792], BF16, st)
            win_t = T()
            msk = sb("msk", [128, 3 * 512], BF16, st)
            msk_t = T()
            S.dma("pool", msk[:], masks, writes=[msk_t], sem_tile=msk_t)
            mask_own = msk[:, 0:512]
            mask_prev = msk[:, 512:1024]
            mask_first = msk[:, 1024:1536]
            S.dma("pool", win[:], w_in.rearrange("(c p) m -> p c m", p=128), writes=[win_t], sem_tile=win_t)
            diag = sb("diag", [128, 4 * 31, 128], BF16, st)
            diag_t = T()
            S.op("dve", lambda h: h.tensor_tensor(
                out=diag[:, :, :], in0=idf[:, :].unsqueeze(1).to_broadcast([128, 124, 128]),
                in1=par_wdw[:, :].unsqueeze(2).to_broadcast([128, 124, 128]), op=ALU.mult),
                reads=[par_t], writes=[diag_t])
            xb = [sb(f"xb{i}", [128, 8, GT], BF16, st) for i in range(2)]
            xb_t = [T() for _ in range(2)]
            hbuf = sb("hbuf", [128, 4, 32 + GT], BF16, st)
            hh_t = [T() for _ in range(4)]
            hb_t = [T() for _ in range(4)]
            qb = sb("qb", [128, 4, GT], BF16, st)
            qb_t = T()
            kA = sb("kA", [128, 128 + GT], BF16, st)
            kB = sb("kB", [128, 128 + GT], BF16, st)
            kh_t, kb_t = T(), T()
            vz = sb("vz", [128, 5, 2, 128], BF16, st)
            vh_t, vb_t = T(), T()
            sg = sb("sg", [128, 2, GT], F32, st)
            sg_t = [T(), T()]
            yc = sb("yc", [128, 4, GT], F32, st)
            yc_t = [T() for _ in range(4)]
            lt = ln_tmps(st, "c", 4)
            pT = sb("pT", [128, 2, 4, GT], BF16, st)
            pT_t = [[T() for _ in range(4)] for _ in range(2)]
            den = sb("den", [128, 2, 2, GT], F32, st)
            den_t = [[T(), T()], [T(), T()]]
            S.op("pool", lambda h: h.memset(kA[:], 0.0), writes=[kh_t, kb_t])
            S.op("pool", lambda h: h.memset(kB[:], 0.0), writes=[kh_t, kb_t])
            S.op("pool", lambda h: h.memset(vz[:], 0.0), writes=[vh_t, vb_t])
            S.op("pool", lambda h: h.memset(hbuf[:], 0.0), writes=hh_t + hb_t)

            def load_x(gi, buf):
                if gi < 0:
                    src = xT[:, 0:HALO]
                    dst = xb[buf][:, :, 0:HALO]
                else:
                    src = xT[:, HALO + gi * GT: HALO + (gi + 1) * GT]
                    dst = xb[buf][:, :, :]
                S.dma("pool", dst, src.rearrange("(c p) t -> p c t", p=128), writes=[xb_t[buf]],
                      sem_tile=xb_t[buf])

            def inproj(col0, buf, nt, pb, pb_t):
                for k in range(8):
                    mm(pb[:, :nt], pb_t, win[:, k, col0:col0 + 128], xb[buf][:, k, :nt],
                       [win_t, xb_t[buf]], k == 0, k == 7)

            load_x(-1, 0)
            for gi in range(-1, NG):
                buf = (gi + 1) % 2
                nt = HALO if gi < 0 else GT
                if gi + 1 < NG:
                    load_x(gi + 1, (gi + 2) % 2)
                hoff = 32 + (GT - nt)
                koff = 128 + (GT - nt)
                if gi >= 0:
                    for j in range(4):
                        if gi == 0:
                            S.op("pool", lambda h, j=j: h.tensor_scalar(hbuf[:, j, 0:32], hbuf[:, j, GT:GT + 32],
                                                                        par_flag[:, 0:1], None, op0=ALU.mult),
                                 reads=[hb_t[j], par_t], writes=[hh_t[j]])
                        else:
                            S.op("pool", lambda h, j=j: h.tensor_copy(out=hbuf[:, j, 0:32],
                                                                      in_=hbuf[:, j, GT:GT + 32]),
                                 reads=[hb_t[j]], writes=[hh_t[j]])
                    S.op("pool", lambda h: h.tensor_copy(out=kA[:, 0:128], in_=kA[:, GT:GT + 128]),
                         reads=[kb_t], writes=[kh_t])
                    S.op("pool", lambda h: h.tensor_copy(out=kB[:, 0:128], in_=kB[:, GT:GT + 128]),
                         reads=[kb_t], writes=[kh_t])
                    S.op("pool", lambda h: h.tensor_copy(out=vz[:, 0, :, :], in_=vz[:, 4, :, :]),
                         reads=[vb_t], writes=[vh_t])
                for j in range(4):
                    pa, pa_t = bank()
                    inproj(j * 128, buf, nt, pa, pa_t)
                    pg, pg_t = bank()
                    inproj((4 + j) * 128, buf, nt, pg, pg_t)
                    s = j % 2
                    S.op("act", lambda h, j=j, s=s, pg=pg, nt=nt: h.activation(
                        out=sg[:, s, :nt], in_=pg[:, :nt], func=AF.Sigmoid, bias=par_b_in[:, 4 + j:5 + j]),
                        reads=[pg_t, par_t], writes=[sg_t[s]])
                    S.op("dve", lambda h, j=j, s=s, pa=pa, nt=nt, hoff=hoff: h.scalar_tensor_tensor(
                        out=hbuf[:, j, hoff:hoff + nt], in0=pa[:, :nt], scalar=par_b_in[:, j:j + 1],
                        in1=sg[:, s, :nt], op0=ALU.add, op1=ALU.mult),
                        reads=[pa_t, sg_t[s], par_t], writes=[hb_t[j]])
                if gi >= 0:
                    for j in range(4):
                        pq, pq_t = bank()
                        inproj((8 + j) * 128, buf, nt, pq, pq_t)
                        S.op("act", lambda h, j=j, pq=pq: h.activation(
                            out=qb[:, j, :], in_=pq[:, :], func=AF.Identity, bias=par_b_in[:, 8 + j:9 + j]),
                            reads=[pq_t, par_t], writes=[qb_t])
                pk, pk_t = bank()
                inproj(12 * 128, buf, nt, pk, pk_t)
                S.op("act", lambda h, pk=pk, nt=nt, koff=koff: h.activation(
                    out=kA[0:64, koff:koff + nt], in_=pk[0:64, :nt], func=AF.Identity, bias=par_b_in[0:64, 12:13]),
                    reads=[pk_t, par_t], writes=[kb_t])
                S.op("act", lambda h, pk=pk, nt=nt, koff=koff: h.activation(
                    out=kB[64:128, koff:koff + nt], in_=pk[64:128, :nt], func=AF.Identity,
                    bias=par_b_in[64:128, 12:13]),
                    reads=[pk_t, par_t], writes=[kb_t])
                nblk = nt // 128
                for b in range(nblk):
                    slot = 1 + b + (4 - nblk)
                    pv, pv_t = bank()
                    for k in range(8):
                        mm(pv[:, 0:128], pv_t, xb[buf][:, k, b * 128:(b + 1) * 128], win[:, k, 13 * 128:14 * 128],
                           [win_t, xb_t[buf]], k == 0, k == 7)
                    S.op("dve", lambda h, pv=pv, slot=slot: h.tensor_tensor(
                        out=vz[:, slot, 0, 0:64], in0=pv[:, 0:64], in1=par_bv[:, 0:64], op=ALU.add),
                        reads=[pv_t, par_t], writes=[vb_t])
                    S.op("dve", lambda h, pv=pv, slot=slot: h.tensor_tensor(
                        out=vz[:, slot, 1, 64:128], in0=pv[:, 64:128], in1=par_bv[:, 64:128], op=ALU.add),
                        reads=[pv_t, par_t], writes=[vb_t])
                if gi < 0:
                    continue
                g = gi
                gc = slice(g * GT, (g + 1) * GT)
                def conv_chunk(j):
                    pc, pc_t = bank()
                    for tap in range(31):
                        mm(pc[:, :], pc_t, diag[:, j * 31 + tap, :], hbuf[:, j, 2 + tap:2 + tap + GT],
                           [diag_t, hh_t[j], hb_t[j]], tap == 0, tap == 30)
                    S.op("act", lambda h, j=j, pc=pc: h.activation(
                        out=yc[:, j, :], in_=pc[:, :], func=AF.Identity, bias=par_cpar[:, j:j + 1]),
                        reads=[pc_t, par_t], writes=[yc_t[j]])
                    ln_pre(lambda c: yc[:, c, :], lambda c: yc_t[c], j, lt)

                def conv_ln():
                    ln_post(lambda c: yc[:, c, :], lambda c: yc_t[c], 4, ones512,
                            [(AF.Silu, lambda c: par_cpar[:, 4 + c:5 + c], lambda c: par_cpar[:, 8 + c:9 + c], [],
                              lambda c, gc=gc: ymix[:, c, gc], lambda c, g=g: ymT[c][g])], lt)
                def scores(b):
                    bp = b % 2
                    first = (g == 0 and b == 0)
                    qv = qb[:, :, b * 128:(b + 1) * 128]
                    for kv in range(2):
                        kX = kA if kv == 0 else kB
                        for po in range(2):
                            ps_, ps_t = bank()
                            kcol = b * 128 + po * 128
                            kt = kh_t if kcol < 128 else kb_t
                            mm(ps_[:, :], ps_t, kX[:, kcol:kcol + 128], qv, [kt, qb_t], True, False)
                            mk = mask_own if po == 1 else (mask_first if first else mask_prev)
                            mm(ps_[:, :], ps_t, ident_b, mk, [cst_t, msk_t], False, True)
                            idx = kv * 2 + po
                            S.op("act", lambda h, ps_=ps_, idx=idx, bp=bp: h.activation(
                                out=pT[:, bp, idx, :], in_=ps_[:, :], func=AF.Exp, scale=0.125),
                                reads=[ps_t], writes=[pT_t[bp][idx]])

                def pv(b):
                    bp = b % 2
                    pTb = pT[:, bp]
                    po_, po_t = bank()
                    pl_, pl_t = bank()
                    n = 0
                    for kv in range(2):
                        for po in range(2):
                            idx = kv * 2 + po
                            slot = b + po
                            vt = vh_t if slot == 0 else vb_t
                            mm(po_[:, :], po_t, vz[:, slot, kv, :], pTb[:, idx, :], [vt, pT_t[bp][idx]], n == 0, n == 3)
                            n += 1
                    n = 0
                    for kv in range(2):
                        for po in range(2):
                            idx = kv * 2 + po
                            mm(pl_[:, :], pl_t, ones0 if kv == 0 else ones1, pTb[:, idx, :], [cst_t, pT_t[bp][idx]],
                               n == 0, n == 3)
                            n += 1
                    S.op("dve", lambda h, pl_=pl_, bp=bp: h.tensor_tensor(
                        out=den[:, bp, 0, :].rearrange("p (g q) -> p g q", g=4),
                        in0=pl_[:, :].rearrange("p (g q) -> p g q", g=4),
                        in1=sinkexp[:, :].unsqueeze(2).to_broadcast([128, 4, 128]), op=ALU.add),
                        reads=[pl_t, sinkexp_t], writes=[den_t[bp][0]])
                    S.op("act", lambda h, bp=bp: h.activation(out=den[:, bp, 0, :], in_=den[:, bp, 0, :], func=AF.Ln),
                         reads=[den_t[bp][0]], writes=[den_t[bp][0]])
                    S.op("act", lambda h, bp=bp: h.activation(out=den[:, bp, 1, :], in_=den[:, bp, 0, :], func=AF.Exp,
                                                              scale=-1.0),
                         reads=[den_t[bp][0]], writes=[den_t[bp][1]])
                    tc0 = g * GT + b * 128
                    S.op("dve", lambda h, po_=po_, tc0=tc0, bp=bp: h.tensor_tensor(
                        out=ymix[:, 4:8, tc0:tc0 + 128], in0=po_[:, :].rearrange("p (g q) -> p g q", g=4),
                        in1=den[:, bp, 1, :].rearrange("p (g q) -> p g q", g=4), op=ALU.mult),
                        reads=[po_t, den_t[bp][1]], writes=[ymT[4 + j][g] for j in range(4)])

                scores(0)
                for b in range(4):
                    conv_chunk(b)
                    if b + 1 < 4:
                        scores(b + 1)
                    pv(b)
                conv_ln()
            S.flush()
        if stop_after == "A1":
            return nc

        kmT = sb("kmT", [128, 8, 256], BF16)
        kmT_t = T(name="kmT")
        vm = sb("vm", [128, 2, 1024], BF16)
        vm_t = T(name="vm")
        x1d_t = [T(name=f"x1d{g}") for g in range(NG)]
        with ExitStack() as st:
            wout = sb("wout", [128, 8, D], BF16, st)
            wout_t = [T() for _ in range(4)]
            for q4 in range(4):
                S.dma("pool", wout[:, :, q4 * 256:(q4 + 1) * 256],
                      w_out[:, q4 * 256:(q4 + 1) * 256].rearrange("(c p) m -> p c m", p=128),
                      writes=[wout_t[q4]], sem_tile=wout_t[q4])
            memb = sb("memb", [128, 8, 256], BF16, st)
            memb_t = T()
            wk = sb("wk", [128, 8, 1024], BF16, st)
            wk_t = T()
            wv = sb("wv", [128, 8, 1024], BF16, st)
            wv_t = T()
            S.dma("pool", memb[:], memT.rearrange("(c p) m -> p c m", p=128), writes=[memb_t], sem_tile=memb_t)
            S.dma("pool", wk[:], w_mkv[:, 0:1024].rearrange("(c p) m -> p c m", p=128), writes=[wk_t], sem_tile=wk_t)
            S.dma("pool", wv[:], w_mkv[:, 1024:2048].rearrange("(c p) m -> p c m", p=128), writes=[wv_t],
                  sem_tile=wv_t)

            def ph0_compute():
                for c in range(8):
                    pb, pb_t = bank()
                    for k in range(8):
                        mm(pb[:, 0:256], pb_t, wk[:, k, colsl(c)], memb[:, k, :], [wk_t, memb_t], k == 0, k == 7)
                    S.op("act", lambda h, c=c, pb=pb: h.activation(out=kmT[:, c, :], in_=pb[:, 0:256], func=AF.Copy),
                         reads=[pb_t], writes=[kmT_t])
                for mc in range(2):
                    for nh in range(2):
                        pb, pb_t = bank()
                        for k in range(8):
                            mm(pb[:, :], pb_t, memb[:, k, colsl(mc)], wv[:, k, nh * 512:(nh + 1) * 512],
                               [wv_t, memb_t], k == 0, k == 7)
                        S.op("dve", lambda h, mc=mc, nh=nh, pb=pb: h.tensor_copy(
                            out=vm[:, mc, nh * 512:(nh + 1) * 512], in_=pb[:, :]),
                            reads=[pb_t], writes=[vm_t])


            xs = [sb(f"xs{i}", [128, 8, GT], F32, st) for i in range(3)]
            xs_l = [T() for _ in range(3)]
            xs_s = [T() for _ in range(3)]
            xs_t = [[T() for _ in range(8)] for _ in range(3)]
            lt = ln_tmps(st, "l1", 8)
            zb1 = sb("l1zb1", [128, 8, GT], BF16, st)
            z2b1 = sb("l1z2b1", [128, 8, GT], BF16, st)
            ltp = [lt, (zb1, [T() for _ in range(8)], z2b1, [T() for _ in range(8)]) + tuple(lt[4:])]

            def a2_stage1(g):
                i = g % 3
                gc = slice(g * GT, (g + 1) * GT)
                S.dma("sp", xs[i][:], xT[:, HALO + g * GT:HALO + (g + 1) * GT].rearrange("(c p) t -> p c t", p=128),
                      writes=xs_t[i], sem_tile=xs_l[i])
                for c in range(8):
                    pb, pb_t = bank()
                    for k in range(8):
                        mm(pb[:, :], pb_t, wout[:, k, colsl(c)], ymix[:, k, gc], [wout_t[c // 2], ymT[k][g]], k == 0, k == 7)
                    S.op("dve", lambda h, c=c, pb=pb, i=i: h.scalar_tensor_tensor(
                        out=xs[i][:, c, :], in0=xs[i][:, c, :], scalar=ALPHA, in1=pb[:, :],
                        op0=ALU.mult, op1=ALU.add),
                        reads=[pb_t, xs_t[i][c]], writes=[xs_t[i][c]])
                    ln_pre(lambda c, i=i: xs[i][:, c, :], lambda c, i=i: xs_t[i][c], c, ltp[g % 2], cast_eng="act")

            a2_stage1(0)
            for g in range(NG):
                i = g % 3
                gc = slice(g * GT, (g + 1) * GT)
                if g + 1 < NG:
                    a2_stage1(g + 1)

                def post1(c, i=i, gc=gc, g=g):
                    S.op("dve", lambda h: h.tensor_copy(out=ymix[:, c, gc], in_=xs[i][:, c, :]),
                         reads=[xs_t[i][c]], writes=[ymT[c][g]])

                ln_post(lambda c, i=i: xs[i][:, c, :], lambda c, i=i: xs_t[i][c], 8, ones1024,
                        [(AF.Identity, lambda c: par_lnp[:, c:c + 1], lambda c: par_lnp[:, 8 + c:9 + c], [],
                          lambda c, i=i: xs[i][:, c, :], lambda c, i=i: xs_t[i][c])], ltp[g % 2], post=post1)
                S.dma("pool", x1T[:, gc].rearrange("(c p) t -> p c t", p=128), xs[i][:],
                      reads=xs_t[i], writes=[x1d_t[g]], sem_tile=xs_s[i])
            ph0_compute()
            S.flush()
        if stop_after == "A2":
            return nc

        acc = sb("acc", [128, 8, TOK], F32)
        accT = [[T(name=f"acc{c}_{g}") for g in range(NG)] for c in range(8)]

        with ExitStack() as st:
            wmq = sb("wmq", [128, 8, D], BF16, st)
            wmq_t = [T() for _ in range(4)]
            wmo = sb("wmo", [128, 8, D], BF16, st)
            wmo_t = [T() for _ in range(4)]
            for wdst, wsrc, wts in ((wmq, w_mq, wmq_t), (wmo, w_mo, wmo_t)):
                for q4 in range(4):
                    S.dma("pool", wdst[:, :, q4 * 256:(q4 + 1) * 256],
                          wsrc[:, q4 * 256:(q4 + 1) * 256].rearrange("(c p) m -> p c m", p=128),
                          writes=[wts[q4]], sem_tile=wts[q4])
            xs1 = sb("xsb", [128, 8, GT], F32, st)
            xs1_l = T()
            xs1_t = [T() for _ in range(8)]
            qm = sb("qm", [128, 8, GT], BF16, st)
            qm_t = [T() for _ in range(8)]
            om, om_t = qm, qm_t
            pmb = sb("pmb", [128, 1, 2, GT], BF16, st)
            pm_t = [[T(), T()]]
            rl = sb("rl", [128, 1, GT], F32, st)
            rl_t = [T()]
            lt = ln_tmps(st, "l2", 8)
            zb1 = sb("l2zb1", [128, 8, GT], BF16, st)
            z2b1 = sb("l2z2b1", [128, 8, GT], BF16, st)
            ltp = [lt, (zb1, [T() for _ in range(8)], z2b1, [T() for _ in range(8)]) + tuple(lt[4:])]

            def b_stage1(g):
                i = g % 2
                gc = slice(g * GT, (g + 1) * GT)
                S.dma("sp", xs1[:], x1T[:, gc].rearrange("(c p) t -> p c t", p=128),
                      reads=[x1d_t[g]], writes=xs1_t, sem_tile=xs1_l)
                for c in range(8):
                    pb, pb_t = bank()
                    for k in range(8):
                        mm(pb[:, :], pb_t, wmq[:, k, colsl(c)], ymix[:, k, gc], [wmq_t[c // 2], ymT[k][g]], k == 0, k == 7)
                    S.op("act", lambda h, c=c, pb=pb: h.activation(out=qm[:, c, :], in_=pb[:, :], func=AF.Copy),
                         reads=[pb_t], writes=[qm_t[c]])
                for hd in range(4):
                    par = 0
                    for mc in range(2):
                        ps_, ps_t = bank()
                        for dc in range(2):
                            mm(ps_[:, :], ps_t, kmT[:, 2 * hd + dc, colsl(mc)], qm[:, 2 * hd + dc, :],
                               [kmT_t, qm_t[2 * hd + dc]], dc == 0, dc == 1)
                        S.op("act", lambda h, ps_=ps_, par=par, mc=mc: h.activation(
                            out=pmb[:, par, mc, :], in_=ps_[:, :], func=AF.Exp, scale=1.0 / 16.0),
                            reads=[ps_t], writes=[pm_t[par][mc]])
                    pl_, pl_t = bank()
                    for mc in range(2):
                        mm(pl_[:, :], pl_t, onesP, pmb[:, par, mc, :], [cst_t, pm_t[par][mc]],
                           mc == 0, mc == 1)
                    S.op("act", lambda h, pl_=pl_, par=par: h.activation(out=rl[:, par, :], in_=pl_[:, :], func=AF.Ln),
                         reads=[pl_t], writes=[rl_t[par]])
                    S.op("act", lambda h, par=par: h.activation(out=rl[:, par, :], in_=rl[:, par, :], func=AF.Exp,
                                                                scale=-1.0),
                         reads=[rl_t[par]], writes=[rl_t[par]])
                    for dc in range(2):
                        po_, po_t = bank()
                        for mc in range(2):
                            mm(po_[:, :], po_t, vm[:, mc, colsl(2 * hd + dc)], pmb[:, par, mc, :],
                               [vm_t, pm_t[par][mc]], mc == 0, mc == 1)
                        S.op("dve", lambda h, po_=po_, par=par, hd=hd, dc=dc: h.tensor_tensor(
                            out=om[:, 2 * hd + dc, :], in0=po_[:, :], in1=rl[:, par, :], op=ALU.mult),
                            reads=[po_t, rl_t[par]], writes=[om_t[2 * hd + dc]])
                for c in range(8):
                    pb, pb_t = bank()
                    for k in range(8):
                        mm(pb[:, :], pb_t, wmo[:, k, colsl(c)], om[:, k, :], [wmo_t[c // 2], om_t[k]], k == 0, k == 7)
                    S.op("dve", lambda h, c=c, pb=pb, gc=gc: h.scalar_tensor_tensor(
                        out=acc[:, c, gc], in0=xs1[:, c, :], scalar=ALPHA, in1=pb[:, :],
                        op0=ALU.mult, op1=ALU.add),
                        reads=[pb_t, xs1_t[c]], writes=[accT[c][g]])
                    ln_pre(lambda c, gc=gc: acc[:, c, gc], lambda c, g=g: accT[c][g], c, ltp[i], cast_eng="act")

            b_stage1(0)
            for g in range(NG):
                i = g % 2
                gc = slice(g * GT, (g + 1) * GT)
                if g + 1 < NG:
                    b_stage1(g + 1)
                def post2(c, gc=gc, g=g):
                    S.op("dve", lambda h: h.tensor_scalar(ymix[:, c, gc], acc[:, c, gc], 1.0 / ALPHA, None, op0=ALU.mult),
                         reads=[accT[c][g]], writes=[ymT[c][g]])

                ln_post(lambda c, gc=gc: acc[:, c, gc], lambda c, g=g: accT[c][g], 8, ones1024,
                        [(AF.Identity, lambda c: par_lnpa[:, c:c + 1], lambda c: par_lnpa[:, 8 + c:9 + c], [lnpa_t],
                          lambda c, gc=gc: acc[:, c, gc], lambda c, g=g: accT[c][g])], ltp[i], post=post2)
            S.flush()
        if stop_after == "B":
            S.dma("sp", outT.rearrange("(c p) t -> p c t", p=128), acc[:],
                  reads=[x for row in accT for x in row], writes=[], sem_tile=T())
            S.flush()
            return nc

        with ExitStack() as st:
            wrt = sb("wrt", [128, 8, 20], F32, st)
            wrt_t = T()
            S.dma("sp", wrt[:], w_rt.rearrange("p (c n) -> p c n", c=8), writes=[wrt_t], sem_tile=wrt_t)
            selt = sb("selt", [16, 16, 128], F32, st)
            selt_t = T()
            S.dma("sp", selt[:], sel.rearrange("k (e m) -> k e m", e=16), writes=[selt_t], sem_tile=selt_t)
            wg = [sb(f"wg{i}", [128, 8, 512], BF16, st) for i in range(2)]
            wu = [sb(f"wu{i}", [128, 8, 512], BF16, st) for i in range(2)]
            wd = [sb(f"wd{i}", [128, 4, D], BF16, st) for i in range(2)]
            wg_t = [T() for _ in range(2)]
            wu_t = [T() for _ in range(2)]
            wd_t = [T() for _ in range(2)]
            cbc = sb("cbc", [128, 2, GT], F32, st)
            cbc_t = [T(), T()]
            sgm = sb("sgm", [128, 2, GT], F32, st)
            sgm_t = [T(), T()]
            tm = sb("tm", [128, 2, GT], F32, st)
            tm_t = [T(), T()]
            hh = sb("hh", [128, 2, 4, GT], BF16, st)
            hh_t2 = [[T() for _ in range(4)] for _ in range(2)]

            def load_expert(e):
                i = e % 2
                S.dma("pool", wg[i][:], w_gate[e].rearrange("(c p) f -> p c f", p=128), writes=[wg_t[i]],
                      sem_tile=wg_t[i])
                S.dma("pool", wu[i][:], w_up[e].rearrange("(c p) f -> p c f", p=128), writes=[wu_t[i]],
                      sem_tile=wu_t[i])
                S.dma("pool", wd[i][:], w_down[e].rearrange("(c p) f -> p c f", p=128), writes=[wd_t[i]],
                      sem_tile=wd_t[i])

            load_expert(0)
            lg = sb("lg", [128, 16, 20], F32, st)
            lg_t = T()
            lgT = wg[1][:].bitcast(F32).rearrange("p c f -> p (c f)")
            for g in range(NG):
                gc = slice(g * GT, (g + 1) * GT)
                pb, pb_t = bank()
                for k in range(8):
                    mm(pb[0:20, :], pb_t, wrt[:, k, :], acc[:, k, gc], [accT[k][g], wrt_t], k == 0, k == 7)
                S.op("act", lambda h, pb=pb, gc=gc: h.activation(
                    out=lgT[0:20, gc], in_=pb[0:20, :], func=AF.Identity, scale=1.0 / ALPHA, bias=par_brc[0:20, 0:1]),
                    reads=[pb_t, par_t], writes=[wg_t[1]])
            for g in range(NG):
                pb, pb_t = bank()
                for q in range(4):
                    tt = g * 4 + q
                    S.op("pe", lambda h, pb=pb, q=q, tt=tt: h.transpose(
                        out=pb[:, q * 20:(q + 1) * 20], in_=lgT[0:20, tt * 128:(tt + 1) * 128],
                        identity=idf[0:20, 0:20]),
                        reads=[wg_t[1], par_t], writes=[pb_t], inc=True)
                S.op("dve", lambda h, pb=pb, g=g: h.tensor_copy(
                    out=lg[:, 4 * g:4 * g + 4, :].rearrange("p a b -> p (a b)"), in_=pb[:, 0:80]),
                    reads=[pb_t], writes=[lg_t])
            R = sb("R", [128, 16, 64], F32, st)
            R_t = T()

            def rop(eng, fn):
                S.op(eng, fn, reads=[lg_t, R_t], writes=[R_t])

            def b3(ap2, n):
                return ap2.unsqueeze(2).to_broadcast([128, 16, n])

            gl = lg[:, :, 0:4]
            gmax, gsum, gp = R[:, :, 0], R[:, :, 1], R[:, :, 2]
            gm = R[:, :, 4:8]
            gd = R[:, :, 8:12]
            el = R[:, :, 12:16]
            tmp = R[:, :, 16:20]
            m1, m2, d21, s1, s2 = R[:, :, 20], R[:, :, 21], R[:, :, 22], R[:, :, 23], R[:, :, 24]
            mk1 = R[:, :, 28:32]
            el2 = R[:, :, 32:36]
            mk2 = R[:, :, 36:40]
            cg = R[:, :, 40:44]
            comb = R[:, :, 48:64]
            rop("dve", lambda h: h.tensor_reduce(out=gmax, in_=gl, axis=AX.X, op=ALU.max))
            rop("dve", lambda h: h.tensor_tensor(out=gm, in0=gl, in1=b3(gmax, 4), op=ALU.is_equal))
            rop("dve", lambda h: h.tensor_tensor(out=gd, in0=gl, in1=b3(gmax, 4), op=ALU.subtract))
            rop("act", lambda h: h.activation(out=gd, in_=gd, func=AF.Exp))
            rop("dve", lambda h: h.tensor_reduce(out=gsum, in_=gd, axis=AX.X, op=ALU.add))
            rop("dve", lambda h: h.reciprocal(out=gp, in_=gsum))
            for gg in range(4):
                dst = el if gg == 0 else tmp
                rop("dve", lambda h, gg=gg, dst=dst: h.tensor_tensor(
                    out=dst, in0=lg[:, :, 4 + 4 * gg:8 + 4 * gg], in1=b3(R[:, :, 4 + gg], 4), op=ALU.mult))
                if gg > 0:
                    rop("dve", lambda h: h.tensor_tensor(out=el, in0=el, in1=tmp, op=ALU.add))
            rop("dve", lambda h: h.tensor_reduce(out=m1, in_=el, axis=AX.X, op=ALU.max))
            rop("dve", lambda h: h.tensor_tensor(out=mk1, in0=el, in1=b3(m1, 4), op=ALU.is_equal))
            rop("dve", lambda h: h.scalar_tensor_tensor(out=el2, in0=mk1, scalar=-1.0e30, in1=el,
                                                        op0=ALU.mult, op1=ALU.add))
            rop("dve", lambda h: h.tensor_reduce(out=m2, in_=el2, axis=AX.X, op=ALU.max))
            rop("dve", lambda h: h.tensor_tensor(out=mk2, in0=el2, in1=b3(m2, 4), op=ALU.is_equal))
            rop("dve", lambda h: h.tensor_tensor(out=d21, in0=m2, in1=m1, op=ALU.subtract))
            rop("act", lambda h: h.activation(out=d21, in_=d21, func=AF.Exp))
            rop("dve", lambda h: h.tensor_scalar(s1, d21, 1.0, None, op0=ALU.add))
            rop("dve", lambda h: h.reciprocal(out=s1, in_=s1))
            rop("dve", lambda h: h.tensor_tensor(out=s2, in0=d21, in1=s1, op=ALU.mult))
            rop("dve", lambda h: h.tensor_tensor(out=s1, in0=s1, in1=gp, op=ALU.mult))
            rop("dve", lambda h: h.tensor_tensor(out=s2, in0=s2, in1=gp, op=ALU.mult))
            rop("dve", lambda h: h.tensor_tensor(out=cg, in0=mk1, in1=b3(s1, 4), op=ALU.mult))
            rop("dve", lambda h: h.tensor_tensor(out=tmp, in0=mk2, in1=b3(s2, 4), op=ALU.mult))
            rop("dve", lambda h: h.tensor_tensor(out=cg, in0=cg, in1=tmp, op=ALU.add))
            for gg in range(4):
                rop("dve", lambda h, gg=gg: h.tensor_tensor(
                    out=R[:, :, 48 + 4 * gg:52 + 4 * gg], in0=cg, in1=b3(R[:, :, 4 + gg], 4), op=ALU.mult))
            combT = sb("combT", [16, TOK], F32, st)
            combT_t = [T() for _ in range(NG)]
            for g in range(NG):
                pb, pb_t = bank()
                for q in range(4):
                    tt = g * 4 + q
                    S.op("pe", lambda h, pb=pb, q=q, tt=tt: h.transpose(
                        out=pb[0:16, q * 128:(q + 1) * 128], in_=R[:, tt, 48:64], identity=idf[:]),
                        reads=[R_t, par_t], writes=[pb_t], inc=True)
                S.op("act", lambda h, pb=pb, g=g: h.activation(
                    out=combT[:, g * GT:(g + 1) * GT], in_=pb[0:16, :], func=AF.Copy),
                    reads=[pb_t], writes=[combT_t[g]])
            ob = sb("ob", [128, 2, 2, GT], F32, st)
            ob_l = [T(), T()]
            ob_t = [T(), T()]
            wdv = wd[0][:].bitcast(F32)
            l3n_t = T()
            lt3 = (wg[0], [T() for _ in range(8)], wu[0], [T() for _ in range(8)], wdv[:, 0, :], T(),
                   wdv[:, 1, :], l3n_t, wdv[:, 1, :], l3n_t, wdv[:, 2:4, :], [T(), T()])
            wdv1 = wd[1][:].bitcast(F32)
            l3n1_t = T()
            lt3b = (wg[1], [T() for _ in range(8)], wu[1], [T() for _ in range(8)], wdv1[:, 0, :], T(),
                    wdv1[:, 1, :], l3n1_t, wdv1[:, 1, :], l3n1_t, wdv1[:, 2:4, :], [T(), T()])
            ln3_state = {0: False, 1: False}

            def ln3_parts(g, which):
                gc = slice(g * GT, (g + 1) * GT)
                tmps = lt3 if which == 0 else lt3b
                z_ap = lambda c, gc=gc: acc[:, c, gc]
                z_t = lambda c, g=g: accT[c][g]

                def pre():
                    if not ln3_state[which]:
                        ln3_state[which] = True
                        allt = [wg_t[which], wu_t[which], wd_t[which]] + tmps[1] + tmps[3] + [tmps[5], tmps[7]] + tmps[11]
                        S.op("pool", lambda h: h.memset(tmps[4][:, 0:2], 0.0), writes=allt)
                    for c in range(8):
                        ln_pre(z_ap, z_t, c, tmps)

                def post(c):
                    if c % 2 == 1:
                        bf = (c // 2) % 2
                        S.dma("sp", outT[(c - 1) * 128:(c + 1) * 128, gc].rearrange("(c p) t -> p c t", p=128),
                              ob[:, bf, :, :], reads=[ob_t[bf]], writes=[], sem_tile=ob_l[bf])

                def fin():
                    ln_post(z_ap, z_t, 8, ones1024,
                            [(AF.Identity, lambda c: par_lnp[:, 32 + c:33 + c], lambda c: par_lnp[:, 40 + c:41 + c], [],
                              lambda c: ob[:, (c // 2) % 2, c % 2, :], lambda c: ob_t[(c // 2) % 2])], tmps, post=post)

                return pre, fin

            def ln3(g):
                pre, fin = ln3_parts(g, 0)
                pre()
                fin()

            def gateup(e, g, ci):
                i = e % 2
                gc = slice(g * GT, (g + 1) * GT)
                pcb, pcb_t = bank()
                mm(pcb[:, :], pcb_t, selt[:, e, :], combT[:, gc], [selt_t, combT_t[g]], True, True)
                S.op("act", lambda h, pcb=pcb, ci=ci: h.activation(out=cbc[:, ci, :], in_=pcb[:, :], func=AF.Copy),
                     reads=[pcb_t], writes=[cbc_t[ci]])
                for fc in range(4):
                    s_ = fc % 2
                    pg_, pg_t = bank()
                    for k in range(8):
                        mm(pg_[:, :], pg_t, wg[i][:, k, colsl(fc)], ymix[:, k, gc], [wg_t[i], ymT[k][g]],
                           k == 0, k == 7)
                    pu_, pu_t = bank()
                    for k in range(8):
                        mm(pu_[:, :], pu_t, wu[i][:, k, colsl(fc)], ymix[:, k, gc], [wu_t[i], ymT[k][g]],
                           k == 0, k == 7)
                    S.op("act", lambda h, pg_=pg_, s_=s_: h.activation(out=sgm[:, s_, :], in_=pg_[:, :], func=AF.Silu),
                         reads=[pg_t], writes=[sgm_t[s_]])
                    S.op("dve", lambda h, pu_=pu_, s_=s_: h.tensor_tensor(
                        out=tm[:, s_, :], in0=sgm[:, s_, :], in1=pu_[:, :], op=ALU.mult),
                        reads=[pu_t, sgm_t[s_]], writes=[tm_t[s_]])
                    S.op("pool", lambda h, s_=s_, ci=ci, fc=fc: h.tensor_tensor(
                        out=hh[:, ci, fc, :], in0=tm[:, s_, :], in1=cbc[:, ci, :], op=ALU.mult),
                        reads=[tm_t[s_], cbc_t[ci]], writes=[hh_t2[ci][fc]])

            def down(e, g, ci):
                i = e % 2
                gc = slice(g * GT, (g + 1) * GT)
                for dc in range(8):
                    pd_, pd_t = bank()
                    for fc in range(4):
                        mm(pd_[:, :], pd_t, wd[i][:, fc, colsl(dc)], hh[:, ci, fc, :], [wd_t[i], hh_t2[ci][fc]],
                           fc == 0, fc == 3)
                    S.op("dve", lambda h, pd_=pd_, dc=dc, gc=gc: h.tensor_tensor(
                        out=acc[:, dc, gc], in0=acc[:, dc, gc], in1=pd_[:, :], op=ALU.add),
                        reads=[pd_t, accT[dc][g]], writes=[accT[dc][g]])
                if e == 15 and g in (1, 2):
                    ln3(g - 1)

            units = [(e, g) for e in range(16) for g in range(NG)]
            for u, (e, g) in enumerate(units):
                gateup(e, g, u % 2)
                if u >= 1:
                    pe_, pg2 = units[u - 1]
                    down(pe_, pg2, (u - 1) % 2)
                if g == 0 and e + 1 < 16:
                    load_expert(e + 1)
            down(15, NG - 1, (len(units) - 1) % 2)
            pre2, fin2 = ln3_parts(NG - 2, 0)
            pre3, fin3 = ln3_parts(NG - 1, 1)
            pre2()
            pre3()
            fin2()
            fin3()
            S.flush()
    return nc


def _chunked(v, n):
    return np.ascontiguousarray(np.asarray(v, np.float32).reshape(n, 128).T)


def prepare_inputs(inp):
    f = lambda k: np.asarray(inp[k], np.float32)
    x, mem = f("x"), f("mem")
    w_in, b_in = f("w_in")[0], f("b_in")[0]
    qperm = np.empty(512, np.int64)
    for j in range(4):
        for r in range(2):
            qperm[j * 128 + r * 64:(j * 128 + r * 64 + 64)] = (r * 4 + j) * 64 + np.arange(64)
    cols = np.concatenate([np.arange(1024), 1024 + qperm, np.arange(1536, 1792)])
    w_in_p = np.ascontiguousarray(w_in[:, cols])
    b_in_p = b_in[cols]
    shared = {
        "w_in": w_in_p,
        "b_in": _chunked(b_in_p, 14),
        "bv_rep": np.ascontiguousarray(np.tile(b_in_p[1664:1792][None, :], (128, 1))),
        "wdw": np.ascontiguousarray(f("w_dw")[0].reshape(31, 4, 128).transpose(2, 1, 0).reshape(128, 124)),
        "cpar": np.concatenate([_chunked(f("b_dw")[0], 4), _chunked(f("g_conv_norm")[0], 4),
                                _chunked(f("b_conv_norm")[0], 4)], axis=1),
        "w_out": np.ascontiguousarray(f("w_out")[0][np.concatenate([np.arange(512), 512 + qperm])]),
        "lnp": np.concatenate([_chunked(f(k)[0], 8) for k in ("g_ln1", "b_ln1", "g_ln2", "b_ln2", "g_ln3", "b_ln3")],
                              axis=1),
        "w_mq": f("w_mq")[0], "w_mkv": f("w_mkv")[0], "w_mo": f("w_mo")[0],
        "w_gate": f("w_gate")[0], "w_up": f("w_up")[0], "w_down": f("w_down")[0],
    }
    sk = f("attn_sinks")[0]
    shared["sinks"] = np.ascontiguousarray(np.concatenate([np.tile(sk[None, 0:4], (64, 1)),
                                                           np.tile(sk[None, 4:8], (64, 1))], axis=0))
    wr = np.concatenate([f("w_group")[0]] + [f("w_router")[0][g] for g in range(4)], axis=1)
    shared["w_rt"] = np.ascontiguousarray(wr.reshape(8, 128, 20).transpose(1, 0, 2).reshape(128, 160))
    br = np.concatenate([f("b_group")[0], f("b_router")[0].reshape(-1)])
    shared["b_rt"] = np.ascontiguousarray(np.tile(br[None, :], (128, 1)))
    shared["b_rt_col"] = np.ascontiguousarray(br.reshape(20, 1))
    kk = np.arange(128)[:, None]
    qq = np.arange(128)[None, :]
    m_own = np.where(kk <= qq, 0.0, NEG).astype(np.float32)
    m_prev = np.where(kk > qq, 0.0, NEG).astype(np.float32)
    m_none = np.full((128, 128), NEG, np.float32)
    eye = np.eye(128, dtype=np.float32)
    o0 = np.zeros((128, 128), np.float32); o0[:, :64] = 1.0
    o1 = np.zeros((128, 128), np.float32); o1[:, 64:] = 1.0
    on = np.ones((128, 128), np.float32)
    shared["consts"] = np.ascontiguousarray(np.concatenate([eye, o0, o1, on / 512.0, on / 1024.0, on], axis=1))
    shared["identf"] = eye
    sel = np.zeros((16, 16, 128), np.float32)
    for e in range(16):
        sel[e, e, :] = 1.0
    shared["sel"] = sel.reshape(16, 2048)
    in_maps = []
    for c in range(NCORES):
        b, s0 = c // 4, (c % 4) * TOK
        halo = x[b, s0 - HALO:s0] if s0 > 0 else np.zeros((HALO, D), np.float32)
        m = dict(shared)
        m["xT"] = np.ascontiguousarray(np.concatenate([halo, x[b, s0:s0 + TOK]], axis=0).T)
        m["flag"] = np.full((128, 1), 1.0 if s0 > 0 else 0.0, np.float32)
        m["memT"] = np.ascontiguousarray(mem[b].T)
        mf = m_prev if s0 > 0 else m_none
        m["masks"] = np.ascontiguousarray(np.concatenate([np.tile(m_own, (1, 4)), np.tile(m_prev, (1, 4)),
                                                          np.tile(mf, (1, 4))], axis=1))
        in_maps.append(m)
    return in_maps


_NC_CACHE = {}


def kernel(**inputs):
    in_maps = prepare_inputs(inputs)
    if "nc" not in _NC_CACHE:
        _NC_CACHE["nc"] = build_program()
    nc = _NC_CACHE["nc"]
    res = run_bass_kernel_spmd(nc, in_maps, core_ids=list(range(NCORES)))
    out = np.empty((2, SEQ, D), np.float32)
    for c in range(NCORES):
        b, s0 = c // 4, (c % 4) * TOK
        out[b, s0:s0 + TOK, :] = np.asarray(res.results[c]["outT"], np.float32).T
    return out
```
